# Optimizing a Trainium2 kernel written in Bass

```python
import jax, jax.numpy as jnp
from jax import lax
import numpy as np

D_MODEL = 1024
BATCH = 1
SEQ = 16384
DEPTH = 4

GRID_W = 64
CTX_LEN = 256
N_MIXERS = 4
GROUP_W = D_MODEL // N_MIXERS
HEAD_DIM = 64
N_GROUP_HEADS = GROUP_W // HEAD_DIM
NA_KH = 8
NA_KW = 16
GQA_KV_HEADS = N_GROUP_HEADS // 2
MLA_Q_LORA = GROUP_W
MLA_KV_LORA = GROUP_W // 2
MLA_NOPE = HEAD_DIM
MLA_ROPE = HEAD_DIM // 2
MLA_V = HEAD_DIM
LRU_BLOCKS = N_GROUP_HEADS
LRU_BW = GROUP_W // LRU_BLOCKS
LRU_CONV = 4
LRU_C = 8.0
N_EXPERTS = 32
TOP_K = 4
D_EXPERT = D_MODEL
SWIGLU_LIMIT = 7.0
SWIGLU_ALPHA = 1.702
EXPERT_BLOCK = 128
Q_BLOCK = 128
ROPE_THETA = 10000.0
EPS = 1e-6
IN_SIZES = (GROUP_W, GROUP_W, GROUP_W,
            GROUP_W, GQA_KV_HEADS * HEAD_DIM, GQA_KV_HEADS * HEAD_DIM,
            MLA_Q_LORA, MLA_KV_LORA, MLA_ROPE,
            GROUP_W, GROUP_W)
IN_COLS = sum(IN_SIZES)

kernel_name = "hybrid_parallel_heads_dit_moe"


def rmsnorm(x, g):
    xf = x.astype(jnp.float32)
    y = xf * lax.rsqrt(jnp.mean(xf * xf, axis=-1, keepdims=True) + EPS)
    return y.astype(x.dtype) * g


def heads(x, n):
    B, N, _ = x.shape
    return x.reshape(B, N, n, -1).transpose(0, 2, 1, 3)


def merge_heads(x):
    B, H, N, d = x.shape
    return x.transpose(0, 2, 1, 3).reshape(B, N, H * d)


def split_cols(u):
    offs, o = [], 0
    for s in IN_SIZES[:-1]:
        o += s
        offs.append(o)
    return jnp.split(u, offs, axis=-1)


def grid_angles(n_tok, rot_dim):
    t = jnp.arange(n_tok)
    row = (t // GRID_W).astype(jnp.float32)
    col = (t % GRID_W).astype(jnp.float32)
    ax = rot_dim // 2
    inv = ROPE_THETA ** (-jnp.arange(0, ax, 2, dtype=jnp.float32) / ax)
    return jnp.concatenate([row[:, None] * inv, col[:, None] * inv], axis=-1)


def rope_2d(x, ang):
    h = x.shape[-1] // 4
    cos = jnp.cos(ang).astype(x.dtype).reshape(-1, 2, h)
    sin = jnp.sin(ang).astype(x.dtype).reshape(-1, 2, h)
    xr = x.reshape(*x.shape[:-1], 2, 2, h)
    x1, x2 = xr[..., 0, :], xr[..., 1, :]
    out = jnp.stack([x1 * cos - x2 * sin, x2 * cos + x1 * sin], axis=-2)
    return out.reshape(x.shape)


def blocked_attention(q, k, v, scale):
    B, KH, G, N, d = q.shape
    qb = min(Q_BLOCK, N)
    qs = q.reshape(B, KH, G, N // qb, qb, d).transpose(3, 0, 1, 2, 4, 5)

    def one(qblk):
        s = jnp.einsum('bkgqd,bkmd->bkgqm', qblk, k).astype(jnp.float32) * scale
        p = jax.nn.softmax(s, axis=-1).astype(v.dtype)
        return jnp.einsum('bkgqm,bkmv->bkgqv', p, v)

    o = lax.map(one, qs)
    return o.transpose(1, 2, 3, 0, 4, 5).reshape(B, KH, G, N, v.shape[-1])


def neighbourhood_attention(q, k, v, kc, vc, rpb, rows, scale):
    B, H, S, d = q.shape
    kh = min(NA_KH, rows)
    t = jnp.arange(S)
    r, cidx = t // GRID_W, t % GRID_W
    r0 = jnp.clip(r - kh // 2, 0, rows - kh)
    c0 = jnp.clip(cidx - NA_KW // 2, 0, GRID_W - NA_KW)
    kr = r0[:, None] + jnp.arange(kh)[None, :]
    kcol = c0[:, None] + jnp.arange(NA_KW)[None, :]
    nbr = (kr[:, :, None] * GRID_W + kcol[:, None, :]).reshape(S, kh * NA_KW)
    rel = ((kr - r[:, None] + NA_KH - 1)[:, :, None] * (2 * NA_KW - 1)
           + (kcol - cidx[:, None] + NA_KW - 1)[:, None, :]).reshape(S, kh * NA_KW)
    rpb_flat = rpb.reshape(H, -1)
    nb = S // Q_BLOCK
    n_key = kh * NA_KW
    qs = q.reshape(B, H, nb, Q_BLOCK, d).transpose(2, 0, 1, 3, 4)

    def one(args):
        qblk, idx, ridx = args
        kn = k[:, :, idx]
        vn = v[:, :, idx]
        s_nb = (jnp.einsum('bhqd,bhqkd->bhqk', qblk, kn).astype(jnp.float32) * scale
                + rpb_flat[:, ridx].astype(jnp.float32))
        s_cx = jnp.einsum('bhqd,bhcd->bhqc', qblk, kc).astype(jnp.float32) * scale
        p = jax.nn.softmax(jnp.concatenate([s_nb, s_cx], axis=-1), axis=-1).astype(v.dtype)
        return (jnp.einsum('bhqk,bhqkd->bhqd', p[..., :n_key], vn)
                + jnp.einsum('bhqc,bhcd->bhqd', p[..., n_key:], vc))

    o = lax.map(one, (qs, nbr.reshape(nb, Q_BLOCK, n_key), rel.reshape(nb, Q_BLOCK, n_key)))
    return o.transpose(1, 2, 0, 3, 4).reshape(B, H, S, d)


def mla_q(cq, qa_g, wuq, qn, ang):
    q = heads(rmsnorm(cq, qa_g) @ wuq, N_GROUP_HEADS)
    q_nope = rmsnorm(q[..., :MLA_NOPE], qn[:MLA_NOPE])
    q_rope = rmsnorm(q[..., MLA_NOPE:], qn[MLA_NOPE:])
    if ang is not None:
        q_rope = rope_2d(q_rope, ang)
    return jnp.concatenate([q_nope, q_rope], axis=-1)


def mla_kv(ckv, kr, kva_g, wukv, kn, ang):
    kv = heads(rmsnorm(ckv, kva_g) @ wukv, N_GROUP_HEADS)
    k_nope = rmsnorm(kv[..., :MLA_NOPE], kn[:MLA_NOPE])
    v = kv[..., MLA_NOPE:]
    k_rope = rmsnorm(kr, kn[MLA_NOPE:])[:, None]
    if ang is not None:
        k_rope = rope_2d(k_rope, ang)
    k_rope = jnp.broadcast_to(k_rope, k_nope.shape[:-1] + (MLA_ROPE,))
    return jnp.concatenate([k_nope, k_rope], axis=-1), v


def conv_centred(x, w, b):
    y = lax.conv_general_dilated(x, w, window_strides=(1,),
                                 padding=[((LRU_CONV - 1) // 2, LRU_CONV // 2)],
                                 dimension_numbers=('NWC', 'WIO', 'NWC'),
                                 feature_group_count=x.shape[-1])
    return y + b


def rglru_coeffs(xb, wa, ba, wi, bi, lam):
    B, N, W = xb.shape
    xg = xb.reshape(B, N, LRU_BLOCKS, LRU_BW)
    r = jax.nn.sigmoid(jnp.einsum('bngi,gij->bngj', xg, wa).reshape(B, N, W) + ba)
    i = jax.nn.sigmoid(jnp.einsum('bngi,gij->bngj', xg, wi).reshape(B, N, W) + bi)
    log_a = -LRU_C * r.astype(jnp.float32) * jax.nn.softplus(-lam.astype(jnp.float32))
    a = jnp.exp(log_a)
    mult = jnp.sqrt(-jnp.expm1(2.0 * log_a))
    return a, mult * (i * xb).astype(jnp.float32)


def linear_scan(a, b, h0):
    b = b.at[:, 0].add(a[:, 0] * h0)

    def comb(e1, e2):
        a1, b1 = e1
        a2, b2 = e2
        return a1 * a2, a2 * b1 + b2

    _, h = lax.associative_scan(comb, (a, b), axis=1)
    return h


def rglru_bidirectional(xl, xc, wa, ba, wi, bi, lam, with_ctx):
    B, S, W = xl.shape
    outs_l, outs_c = [], []
    for d in range(2):
        rev = (lambda z: jnp.flip(z, axis=1)) if d else (lambda z: z)
        a_c, b_c = rglru_coeffs(rev(xc), wa[d], ba[d], wi[d], bi[d], lam[d])
        h_c = linear_scan(a_c, b_c, jnp.zeros((B, W), jnp.float32))
        a_l, b_l = rglru_coeffs(rev(xl), wa[d], ba[d], wi[d], bi[d], lam[d])
        h_l = linear_scan(a_l, b_l, h_c[:, -1])
        outs_l.append(rev(h_l))
        outs_c.append(rev(h_c))
    y_l = (outs_l[0] + outs_l[1]).astype(xl.dtype)
    y_c = (outs_c[0] + outs_c[1]).astype(xc.dtype) if with_ctx else None
    return y_l, y_c


def merge_groups(parts, grp_g, w_out):
    y = jnp.concatenate(parts, axis=-1)
    B, N, _ = y.shape
    y = rmsnorm(y.reshape(B, N, N_MIXERS, GROUP_W), grp_g.reshape(N_MIXERS, GROUP_W))
    return y.reshape(B, N, D_MODEL) @ w_out


def token_mixers(h, hc, w_in, na_qn, na_kn, na_rpb, gqa_qn, gqa_kn,
                 mla_qa_g, mla_kva_g, mla_wuq, mla_wukv, mla_qn, mla_kn,
                 lru_conv_w, lru_conv_b, lru_wa, lru_ba, lru_wi, lru_bi, lru_lam,
                 grp_g, w_out, with_ctx):
    B, S, _ = h.shape
    rows = S // GRID_W
    H, KVH = N_GROUP_HEADS, GQA_KV_HEADS
    sc = HEAD_DIM ** -0.5
    na_q, na_k, na_v, g_q, g_k, g_v, m_cq, m_ckv, m_kr, l_x, l_gate = split_cols(h @ w_in)
    na_qc, na_kc, na_vc, g_qc, g_kc, g_vc, m_cqc, m_ckvc, m_krc, l_xc, l_gatec = split_cols(hc @ w_in)

    kac = rmsnorm(heads(na_kc, H), na_kn)
    vac = heads(na_vc, H)
    o_a = neighbourhood_attention(rmsnorm(heads(na_q, H), na_qn), rmsnorm(heads(na_k, H), na_kn),
                                  heads(na_v, H), kac, vac, na_rpb, rows, sc)

    ang = grid_angles(S, HEAD_DIM)
    qb = rope_2d(rmsnorm(heads(g_q, H), gqa_qn), ang)
    kbc = rmsnorm(heads(g_kc, KVH), gqa_kn)
    vbc = heads(g_vc, KVH)
    kb_all = jnp.concatenate([rope_2d(rmsnorm(heads(g_k, KVH), gqa_kn), ang), kbc], axis=2)
    vb_all = jnp.concatenate([heads(g_v, KVH), vbc], axis=2)
    o_b = blocked_attention(qb.reshape(B, KVH, H // KVH, S, HEAD_DIM), kb_all, vb_all, sc)
    o_b = o_b.reshape(B, H, S, HEAD_DIM)

    ang_m = grid_angles(S, MLA_ROPE)
    sc_m = (MLA_NOPE + MLA_ROPE) ** -0.5
    qm = mla_q(m_cq, mla_qa_g, mla_wuq, mla_qn, ang_m)
    km, vm = mla_kv(m_ckv, m_kr, mla_kva_g, mla_wukv, mla_kn, ang_m)
    kmc, vmc = mla_kv(m_ckvc, m_krc, mla_kva_g, mla_wukv, mla_kn, None)
    o_c = blocked_attention(qm[:, :, None], jnp.concatenate([km, kmc], axis=2),
                            jnp.concatenate([vm, vmc], axis=2), sc_m).reshape(B, H, S, MLA_V)

    y_l, y_c = rglru_bidirectional(conv_centred(l_x, lru_conv_w, lru_conv_b),
                                   conv_centred(l_xc, lru_conv_w, lru_conv_b),
                                   lru_wa, lru_ba, lru_wi, lru_bi, lru_lam, with_ctx)
    o_d = y_l * jax.nn.gelu(l_gate)

    y = merge_groups([merge_heads(o_a), merge_heads(o_b), merge_heads(o_c), o_d], grp_g, w_out)
    if not with_ctx:
        return y, None
    C = hc.shape[1]
    oac = blocked_attention(rmsnorm(heads(na_qc, H), na_qn)[:, :, None], kac, vac, sc).reshape(B, H, C, HEAD_DIM)
    obc = blocked_attention(rmsnorm(heads(g_qc, H), gqa_qn).reshape(B, KVH, H // KVH, C, HEAD_DIM),
                            kbc, vbc, sc).reshape(B, H, C, HEAD_DIM)
    qmc = mla_q(m_cqc, mla_qa_g, mla_wuq, mla_qn, None)
    occ = blocked_attention(qmc[:, :, None], kmc, vmc, sc_m).reshape(B, H, C, MLA_V)
    odc = y_c * jax.nn.gelu(l_gatec)
    yc = merge_groups([merge_heads(oac), merge_heads(obc), merge_heads(occ), odc], grp_g, w_out)
    return y, yc


def moe_ffn(h, router_w, router_b, w_gu, b_gu, w_down, b_down):
    T, D = h.shape
    logits = (h @ router_w + router_b).astype(jnp.float32)
    top_val, top_idx = lax.top_k(logits, TOP_K)
    gates = jax.nn.softmax(top_val, axis=-1)
    M = T * TOP_K
    e_flat = top_idx.reshape(M)
    tok_flat = jnp.arange(M, dtype=jnp.int32) // TOP_K
    order = jnp.argsort(e_flat)
    e_s, tok_s = e_flat[order], tok_flat[order]
    g_s = gates.reshape(M)[order].astype(h.dtype)
    counts = jnp.bincount(e_flat, length=N_EXPERTS)
    padded = (counts + EXPERT_BLOCK - 1) // EXPERT_BLOCK * EXPERT_BLOCK
    pad_end = jnp.cumsum(padded)
    pad_start = pad_end - padded
    start = jnp.cumsum(counts) - counts
    pos = pad_start[e_s] + jnp.arange(M, dtype=jnp.int32) - start[e_s]
    n_blocks = -(-(M + N_EXPERTS * (EXPERT_BLOCK - 1)) // EXPERT_BLOCK)
    P = n_blocks * EXPERT_BLOCK
    row_tok = jnp.full((P,), T, jnp.int32).at[pos].set(tok_s)
    row_gate = jnp.zeros((P,), h.dtype).at[pos].set(g_s)
    blk_expert = jnp.minimum(jnp.searchsorted(pad_end, jnp.arange(n_blocks) * EXPERT_BLOCK, side='right'),
                             N_EXPERTS - 1)
    h_pad = jnp.concatenate([h, jnp.zeros((1, D), h.dtype)], axis=0)
    xin = h_pad[row_tok].reshape(n_blocks, EXPERT_BLOCK, D)

    def expert_block(args):
        xb, e = args
        gu = xb @ w_gu[e] + b_gu[e]
        x_glu = jnp.minimum(gu[:, :D_EXPERT], SWIGLU_LIMIT)
        x_lin = jnp.clip(gu[:, D_EXPERT:], -SWIGLU_LIMIT, SWIGLU_LIMIT)
        act = x_glu * jax.nn.sigmoid(SWIGLU_ALPHA * x_glu) * (x_lin + 1)
        return act @ w_down[e] + b_down[e]

    y = lax.map(expert_block, (xin, blk_expert)).reshape(P, D) * row_gate[:, None]
    return jax.ops.segment_sum(y, row_tok, num_segments=T + 1)[:T]


def setup_inputs(seed: int = 0) -> dict:
    key = jax.random.key(seed)
    ks = jax.random.split(key, 36)

    def nrm(i, shape, scale):
        return jax.random.normal(ks[i], shape, jnp.float32) * scale

    def gain(i, shape):
        return 1.0 + 0.1 * jax.random.normal(ks[i], shape, jnp.float32)

    L, D, H = DEPTH, D_MODEL, N_GROUP_HEADS
    s = D ** -0.5
    a_base = jax.random.uniform(ks[26], (L, 2, GROUP_W), jnp.float32, 0.9, 0.999) ** (1.0 / LRU_C)
    return {
        "x": nrm(0, (BATCH, SEQ, D), 1.0),
        "c": nrm(1, (BATCH, D), 1.0),
        "ctx": nrm(2, (BATCH, CTX_LEN, D), 1.0),
        "c_ctx": nrm(3, (D,), 1.0),
        "ada_w": nrm(4, (L, D, 6 * D), 0.5 * s),
        "ada_b": nrm(5, (L, 6 * D), 0.02),
        "norm1_g": gain(6, (L, D)),
        "norm2_g": gain(7, (L, D)),
        "w_in": nrm(8, (L, D, IN_COLS), s),
        "na_qn": gain(9, (L, HEAD_DIM)),
        "na_kn": gain(10, (L, HEAD_DIM)),
        "na_rpb": nrm(11, (L, H, 2 * NA_KH - 1, 2 * NA_KW - 1), 0.5),
        "gqa_qn": gain(12, (L, HEAD_DIM)),
        "gqa_kn": gain(13, (L, HEAD_DIM)),
        "mla_qa_g": gain(14, (L, MLA_Q_LORA)),
        "mla_kva_g": gain(15, (L, MLA_KV_LORA)),
        "mla_wuq": nrm(16, (L, MLA_Q_LORA, H * (MLA_NOPE + MLA_ROPE)), MLA_Q_LORA ** -0.5),
        "mla_wukv": nrm(17, (L, MLA_KV_LORA, H * (MLA_NOPE + MLA_V)), MLA_KV_LORA ** -0.5),
        "mla_qn": gain(18, (L, MLA_NOPE + MLA_ROPE)),
        "mla_kn": gain(19, (L, MLA_NOPE + MLA_ROPE)),
        "lru_conv_w": nrm(20, (L, LRU_CONV, 1, GROUP_W), LRU_CONV ** -0.5),
        "lru_conv_b": nrm(21, (L, GROUP_W), 0.02),
        "lru_wa": nrm(22, (L, 2, LRU_BLOCKS, LRU_BW, LRU_BW), LRU_BW ** -0.5),
        "lru_ba": nrm(23, (L, 2, GROUP_W), 0.02),
        "lru_wi": nrm(24, (L, 2, LRU_BLOCKS, LRU_BW, LRU_BW), LRU_BW ** -0.5),
        "lru_bi": nrm(25, (L, 2, GROUP_W), 0.02),
        "lru_lam": jnp.log(a_base) - jnp.log1p(-a_base),
        "grp_g": gain(27, (L, D)),
        "w_out": nrm(28, (L, D, D), s),
        "router_w": nrm(29, (L, D, N_EXPERTS), s),
        "router_b": nrm(30, (L, N_EXPERTS), 0.01),
        "exp_w_gu": nrm(31, (L, N_EXPERTS, D, 2 * D_EXPERT), s),
        "exp_b_gu": nrm(32, (L, N_EXPERTS, 2 * D_EXPERT), 0.02),
        "exp_w_down": nrm(33, (L, N_EXPERTS, D_EXPERT, D), D_EXPERT ** -0.5),
        "exp_b_down": nrm(34, (L, N_EXPERTS, D), 0.02),
    }


def reference(x, c, ctx, c_ctx, ada_w, ada_b, norm1_g, norm2_g, w_in, na_qn, na_kn, na_rpb,
              gqa_qn, gqa_kn, mla_qa_g, mla_kva_g, mla_wuq, mla_wukv, mla_qn, mla_kn,
              lru_conv_w, lru_conv_b, lru_wa, lru_ba, lru_wi, lru_bi, lru_lam, grp_g, w_out,
              router_w, router_b, exp_w_gu, exp_b_gu, exp_w_down, exp_b_down):
    B, S, D = x.shape
    xc = ctx
    for l in range(DEPTH):
        with_ctx = l < DEPTH - 1
        mod = (jax.nn.silu(c) @ ada_w[l] + ada_b[l])[:, None, :]
        mod_c = (jax.nn.silu(c_ctx) @ ada_w[l] + ada_b[l])[None, None, :]
        sh1, sc1, gt1, sh2, sc2, gt2 = jnp.split(mod, 6, axis=-1)
        csh1, csc1, cgt1, csh2, csc2, cgt2 = jnp.split(mod_c, 6, axis=-1)
        h = rmsnorm(x, norm1_g[l]) * (1 + sc1) + sh1
        hc = rmsnorm(xc, norm1_g[l]) * (1 + csc1) + csh1
        y, yc = token_mixers(h, hc, w_in[l], na_qn[l], na_kn[l], na_rpb[l], gqa_qn[l], gqa_kn[l],
                             mla_qa_g[l], mla_kva_g[l], mla_wuq[l], mla_wukv[l], mla_qn[l], mla_kn[l],
                             lru_conv_w[l], lru_conv_b[l], lru_wa[l], lru_ba[l], lru_wi[l], lru_bi[l],
                             lru_lam[l], grp_g[l], w_out[l], with_ctx)
        x = x + gt1 * y
        tokens = (rmsnorm(x, norm2_g[l]) * (1 + sc2) + sh2).reshape(B * S, D)
        if with_ctx:
            xc = xc + cgt1 * yc
            h2c = rmsnorm(xc, norm2_g[l]) * (1 + csc2) + csh2
            tokens = jnp.concatenate([tokens, h2c.reshape(-1, D)], axis=0)
        f = moe_ffn(tokens, router_w[l], router_b[l], exp_w_gu[l], exp_b_gu[l], exp_w_down[l], exp_b_down[l])
        x = x + gt2 * f[:B * S].reshape(B, S, D)
        if with_ctx:
            xc = xc + cgt2 * f[B * S:].reshape(xc.shape)
    return x
```

```python
import contextlib
import numpy as np
import concourse.bass as bass
import concourse.mybir as mybir
from concourse.bass_utils import run_bass_kernel_spmd

F32 = mybir.dt.float32; BF16 = mybir.dt.bfloat16; U32 = mybir.dt.uint32; I32 = mybir.dt.int32
AF = mybir.ActivationFunctionType; ALU = mybir.AluOpType; AX = mybir.AxisListType

NCORE = 8; D = 1024; S = 16384; TL = 2048; CT = 256; TT = TL + CT; NTL = 16; NT = 18; L = 4
GW = 64; EPS = 1e-6
NE = 32; ELOC = 4; CAP = 2560; NTOK_ALL = NCORE * TT
NTILE_ALL = NTOK_ALL // 128
BW = 512


class Trk:
    __slots__ = ("w", "r")
    def __init__(self):
        self.w = None; self.r = []


class KB:
    ENG = ("pe", "act", "dve", "pool", "sp")
    SEMKEYS = ("c_pe", "c_act", "c_dve", "c_pool", "d_sp", "d_pool", "d_cc")
    def __init__(self, nc):
        self.nc = nc
        self.streams = {e: [] for e in self.ENG}
        self.cnt = {e: 0 for e in self.ENG}
        self.dcnt = {"sp": 0, "pool": 0, "cc": 0}
        self.seen = {e: {} for e in self.ENG}
    def _needs(self, reads, writes):
        ev = []
        for t in reads:
            if t.w is not None: ev.append(t.w)
        for t in writes:
            if t.w is not None: ev.append(t.w)
            ev.extend(t.r)
        return ev
    def _waits(self, e, events):
        need = {}
        for (sk, val, src) in events:
            if src == "pe" and e == "pe" and sk == "c_pe": continue
            if self.seen[e].get(sk, 0) >= val: continue
            if need.get(sk, 0) < val: need[sk] = val
        for sk, val in need.items(): self.seen[e][sk] = val
        return list(need.items())
    def _upd(self, evt, reads, writes):
        for t in writes:
            t.w = evt; t.r = []
        for t in reads:
            if t not in writes: t.r.append(evt)
    def op(self, e, fn, reads=(), writes=()):
        waits = self._waits(e, self._needs(reads, writes))
        self.cnt[e] += 1
        sk = "c_" + e
        evt = (sk, self.cnt[e], e)
        self.streams[e].append((waits, fn, (sk, 1)))
        self._upd(evt, reads, writes)
    def dma(self, q, fn, reads=(), writes=(), cc=False):
        waits = self._waits(q, self._needs(reads, writes))
        key = "cc" if cc else q
        inc = 1 if cc else 16
        self.dcnt[key] += inc
        sk = "d_" + key
        evt = (sk, self.dcnt[key], q)
        self.streams[q].append((waits, fn, (sk, inc)))
        self.streams[q].append(([(sk, self.dcnt[key])], None, None))
        self.seen[q][sk] = self.dcnt[key]
        self._upd(evt, reads, writes)
    def wait_all(self, e, trks):
        ev = []
        for t in trks:
            if t.w is not None: ev.append(t.w)
            ev.extend(t.r)
        waits = self._waits(e, ev)
        if waits: self.streams[e].append((waits, None, None))
    def emit(self, sems):
        nc = self.nc
        with nc.Block() as block:
            def run(eng, lst):
                for waits, fn, inc in lst:
                    for sk, val in waits: eng.wait_ge(sems[sk], val)
                    if fn is not None:
                        ins = fn(eng)
                        ins.then_inc(sems[inc[0]], inc[1])
            if self.streams["pe"]:
                @block.tensor
                def _(eng): run(eng, self.streams["pe"])
            if self.streams["act"]:
                @block.scalar
                def _(eng): run(eng, self.streams["act"])
            if self.streams["dve"]:
                @block.vector
                def _(eng): run(eng, self.streams["dve"])
            if self.streams["pool"]:
                @block.gpsimd
                def _(eng): run(eng, self.streams["pool"])
            if self.streams["sp"]:
                @block.sync
                def _(eng): run(eng, self.streams["sp"])


class Til:
    def __init__(self, t):
        self.t = t; self.k = Trk()
    def __getitem__(self, idx):
        return self.t[idx]


def _rope_tables(n_tok_start, n_tok, rot_dim):
    t = np.arange(n_tok_start, n_tok_start + n_tok)
    row = (t // GW).astype(np.float64); col = (t % GW).astype(np.float64)
    ax = rot_dim // 2
    inv = 10000.0 ** (-np.arange(0, ax, 2, dtype=np.float64) / ax)
    h = rot_dim // 4
    cos = np.zeros((rot_dim, n_tok)); sin = np.zeros((rot_dim, n_tok))
    for d in range(rot_dim):
        axis = d // (2 * h); half = (d // h) % 2; j = d % h
        ang = (row if axis == 0 else col) * inv[j]
        cos[d] = np.cos(ang.astype(np.float32).astype(np.float64))
        sin[d] = np.sin(ang.astype(np.float32).astype(np.float64)) * (-1.0 if half == 0 else 1.0)
    return cos.astype(np.float32), sin.astype(np.float32)


def _rope_perm(rot_dim, n, base=0, reps=1, stride=None):
    P = np.zeros((n, n), np.float32)
    h = rot_dim // 4
    stride = stride or rot_dim
    for r in range(reps):
        for d in range(rot_dim):
            half = (d // h) % 2
            pd = d + h if half == 0 else d - h
            P[base + r * stride + pd, base + r * stride + d] = 1.0
    return P


class Packer:
    def __init__(self):
        self.items = {}; self.parts = []; self.off = 0
    def add(self, name, arr):
        a = np.ascontiguousarray(np.asarray(arr, dtype=np.float32))
        self.items[name] = (self.off, tuple(a.shape))
        self.parts.append(a.reshape(-1)); self.off += a.size
        pad = (-self.off) % 64
        if pad:
            self.parts.append(np.zeros(pad, np.float32)); self.off += pad
    def finish_single(self):
        rows = -(-self.off // BW)
        flat = np.concatenate(self.parts + [np.zeros(rows * BW - self.off, np.float32)])
        return flat.reshape(rows, BW), rows
    def finish(self):
        per = -(-self.off // (NCORE * BW))
        tot = per * NCORE * BW
        flat = np.concatenate(self.parts + [np.zeros(tot - self.off, np.float32)])
        return flat.reshape(NCORE, per, BW), per


CP = {}
def _cp_layout():
    names = [("na_qn2", 1), ("gqa_qn2", 1), ("gqa_kn2", 1), ("mla_qa_g", 2), ("mla_kva_g", 1), ("mla_qn", 1),
             ("mla_knn2", 1), ("mla_knr", 1), ("conv_w", 8), ("conv_b", 2), ("lru_ba", 4), ("lru_bi", 4),
             ("lru_lam", 4), ("grp_g", 8), ("sc96", 1)]
    o = 0
    for n, w in names:
        CP[n] = (o, w); o += w
    return o
NCOL = _cp_layout()


def add_consts(pk):
    ones64 = np.kron(np.eye(2, dtype=np.float32), np.ones((64, 64), np.float32))
    pk.add("ones64blk", ones64)
    pk.add("ones128", np.ones((128, 128), np.float32))
    b96 = np.zeros((128, 128), np.float32); b96[:64, :64] = 1; b96[64:96, 64:96] = 1
    pk.add("blk96", b96)
    pk.add("ident", np.eye(128, dtype=np.float32))
    pk.add("antiid", np.eye(128, dtype=np.float32)[::-1])
    pk.add("prot64", _rope_perm(64, 128, 0, 2))
    p96 = np.zeros((128, 128), np.float32); p96[64:96, 64:96] = _rope_perm(32, 32)
    pk.add("prot96", p96)
    pk.add("prot32", np.pad(_rope_perm(32, 32), ((0, 96), (0, 96))))


def make_cp(inp, l):
    cp = np.zeros((128, NCOL), np.float32)
    def put(name, mat):
        o, w = CP[name]
        mat = np.asarray(mat, np.float32)
        if mat.ndim == 1: mat = mat.reshape(-1, 1)
        cp[:mat.shape[0], o:o + w] = mat
    put("na_qn2", np.tile(inp["na_qn"][l], 2)); put("gqa_qn2", np.tile(inp["gqa_qn"][l], 2))
    put("gqa_kn2", np.tile(inp["gqa_kn"][l], 2))
    put("mla_qa_g", inp["mla_qa_g"][l].reshape(2, 128).T); put("mla_kva_g", inp["mla_kva_g"][l])
    put("mla_qn", inp["mla_qn"][l]); put("mla_knn2", np.tile(inp["mla_kn"][l][:64], 2))
    put("mla_knr", inp["mla_kn"][l][64:])
    put("grp_g", inp["grp_g"][l].reshape(8, 128).T)
    s96 = np.zeros(128, np.float32); s96[:64] = 1.0 / 64; s96[64:96] = 1.0 / 32; s96[96:] = 1.0
    put("sc96", s96)
    return cp


class Ctx:
    pass


def build(emit_fn, iitems, irows, **kw):
    nc = bass.Bass("TRN2", target_bir_lowering=False)
    kb = KB(nc)
    g = Ctx(); g.nc = nc; g.kb = kb; g.iitems = iitems; g.irows = irows
    st = contextlib.ExitStack()
    g.st = st
    with st:
        sems = {k: st.enter_context(nc.semaphore(k)) for k in KB.SEMKEYS}
        _setup(g)
        emit_fn(g, **kw)
        kb.emit(sems)
    return nc


def _setup(g):
    nc, st = g.nc, g.st
    g.o = _ops(g)
    def sb(name, shape, dt, stack=None):
        return Til((stack or st).enter_context(nc.sbuf_tensor(name, list(shape), dt)))
    def ps(name, shape, dt, stack=None):
        return Til((stack or st).enter_context(nc.psum_tensor(name, list(shape), dt)))
    def dram(name, shape, dt, kind=None):
        return nc.dram_tensor(name, list(shape), dt, kind=kind) if kind else nc.dram_tensor(name, list(shape), dt)
    g.sb, g.ps, g.dram = sb, ps, dram
    g.dk = {}
    g.DK = lambda t: g.dk.setdefault(t.name, Trk())
    g.inp = dram("inp", [g.irows, BW], F32, "ExternalInput")
    def iv(name, dims, extra=0):
        off, _ = g.iitems[name]
        return dap(g.inp, off + extra, dims)
    g.iv = iv
    g.bv = iv
    g.BK = Trk()
    g.outs = {}
    def out(name, shape, dt):
        t = dram(name, shape, dt, "ExternalOutput"); g.outs[name] = t
        return t
    g.out = out


def load_consts(g, names):
    o = g.o
    for nm in names:
        tl = g.sb("c_" + nm, [128, 128], F32)
        o.dma(tl[:, :], g.iv(nm, [[128, 128], [1, 128]]), [], [tl.k])
        setattr(g, {"ident": "ident_f", "ones64blk": "ones64"}.get(nm, nm), tl)
    if "ident" in names:
        g.ident_b = g.sb("c_ident_b", [128, 128], BF16)
        o.cp(g.ident_b[:, :], g.ident_f[:, :], [g.ident_f.k], [g.ident_b.k])


def emit_p0(g, nlayers=L):
    o, sb, ps, iv = g.o, g.sb, g.ps, g.iv
    modo = g.out("mod", [2, nlayers * 768], F32)
    cT = sb("cT", [128, 16], F32); scT = sb("scT", [128, 16], F32)
    aw = sb("aw", [128, 8, 768], F32); ab = sb("ab", [2, nlayers * 768], F32)
    msb = sb("msb", [2, nlayers * 768], F32)
    pm = [ps(f"pm{i}", [128, 512], F32) for i in range(2)]
    o.dma(cT[:, :], iv("cT2", [[16, 128], [1, 16]]), [], [cT.k])
    o.act(scT[:, :], cT[:, :], AF.Silu, [cT.k], [scT.k])
    o.dma(ab[:, :], iv("adab", [[0, 2], [1, nlayers * 768]]), [], [ab.k])
    for l in range(nlayers):
        o.dma(aw[:, :, :], iv("adaw", [[768, 128], [128 * 768, 8], [1, 768]], l * D * 768), [], [aw.k])
        for ci, (c0, cn) in enumerate(((0, 512), (512, 256))):
            for k in range(8):
                o.mm(pm[ci][0:2, 0:cn], scT[:, k:16:8], aw[:, k, c0:c0 + cn], k == 0, k == 7, [scT.k, aw.k], [pm[ci].k])
            o.tt(msb[:, l * 768 + c0:l * 768 + c0 + cn], pm[ci][0:2, 0:cn], ab[:, l * 768 + c0:l * 768 + c0 + cn], ALU.add,
                 [pm[ci].k, ab.k], [msb.k])
    o.dma(modo.ap(), msb[:, :], [msb.k], [g.DK(modo)])


def emit_p1(g):
    load_consts(g, ["ident", "ones64blk", "ones128", "blk96", "prot64", "prot96", "prot32"])
    def load_modvec(dst, l, w, v, q="sp"):
        g.o.dma(dst[:, :], g.iv("modv", [[0, 128], [1, 1024]], (w * 6 + v) * 1024), [], [dst.k], q=q)
    g.load_modvec = load_modvec
    _stage_a(g, "")


def _ops(g):
    kb = g.kb
    def mm(out, lhsT, rhs, start, stop, reads, writes):
        kb.op("pe", lambda e: e.matmul(out, lhsT=lhsT, rhs=rhs, start=start, stop=stop), reads, writes)
    def tr(out, in_, ident, reads, writes):
        kb.op("pe", lambda e: e.transpose(out, in_, ident), reads, writes)
    def act(out, in_, func, reads, writes, eng="act", **kw):
        kb.op(eng, lambda e: e.activation(out=out, in_=in_, func=func, **kw), reads, writes)
    def ts(out, in0, s1, s2, op0, op1, reads, writes, eng="dve"):
        kb.op(eng, lambda e: e.tensor_scalar(out=out, in0=in0, scalar1=s1, scalar2=s2, op0=op0, op1=op1), reads, writes)
    def stt(out, in0, scalar, in1, op0, op1, reads, writes):
        kb.op("dve", lambda e: e.scalar_tensor_tensor(out=out, in0=in0, scalar=scalar, in1=in1, op0=op0, op1=op1), reads, writes)
    def tt(out, in0, in1, op, reads, writes, eng="dve"):
        kb.op(eng, lambda e: e.tensor_tensor(out=out, in0=in0, in1=in1, op=op), reads, writes)
    def cp(out, in_, reads, writes, eng="dve"):
        kb.op(eng, lambda e: e.tensor_copy(out=out, in_=in_), reads, writes)
    def rcp(out, in_, reads, writes):
        kb.op("dve", lambda e: e.reciprocal(out=out, in_=in_), reads, writes)
    def red(out, in_, op, reads, writes):
        kb.op("dve", lambda e: e.tensor_reduce(out=out, in_=in_, axis=AX.X, op=op), reads, writes)
    def ms(ap, val, writes, eng="pool"):
        kb.op(eng, lambda e: e.memset(ap, val), (), writes)
    def dma(out, in_, reads, writes, q="sp", **kw):
        kb.dma(q, lambda e: e.dma_start(out=out, in_=in_, **kw), reads, writes)
    def ag(out_t, in_t, reads, writes, kind="AllGather", op=ALU.bypass):
        kb.dma("pool", lambda e: e.collective_compute(kind, op, replica_groups=[list(range(NCORE))],
                                                       ins=[in_t.ap().opt()], outs=[out_t.ap().opt()]),
               reads, writes, cc=True)
        kb.op("pool", lambda e: e.memset(g.dummy[:, :], 0.0), (), list(writes) + [g.dummy.k])
    o = Ctx()
    o.mm, o.tr, o.act, o.ts, o.stt, o.tt, o.cp, o.rcp, o.red, o.ms, o.dma, o.ag = mm, tr, act, ts, stt, tt, cp, rcp, red, ms, dma, ag
    return o


def dap(t, off, dims):
    return bass.AP(tensor=t, offset=off, ap=[list(d) for d in dims])


DBG_SHAPES = {"GQ": (256, TT), "GK_src": (128, TL), "GKC": (128, CT), "NQ": (256, TT), "MQ": (384, TT), "MKN_src": (256, TL),
              "MKNC": (256, CT), "MKR_src": (32, TL), "MKRC": (32, CT), "LX": (256, TT), "LG": (256, TT), "NAK_src": (TL, 256),
              "NAV_src": (TL, 260), "GV_src": (TL, 130), "MV_src": (TL, 260), "LXH_src": (256, 256), "GK_all": (NCORE * 128, TL),
              "NAK_all": (S, 256), "MV_all": (S, 260), "mod_all": (16, 768), "x_cur": (TT, D), "OT": (D, TT), "F_loc": (TT, D)}
import os
SUB = int(os.environ.get('K_SUB', '0'))
TCH = [(0, 512), (512, 512), (1024, 512), (1536, 512), (2048, 256)]


def _layer_dram(g):
    d = g.out
    g.GQ = d("GQ", [256, TT], BF16); g.GK_src = d("GK_src", [128, TL], BF16); g.GKC = d("GKC", [128, CT], BF16)
    g.NQ = d("NQ", [256, TT], BF16)
    g.MQ = d("MQ", [384, TT], BF16)
    g.MKN_src = d("MKN_src", [256, TL], BF16); g.MKNC = d("MKNC", [256, CT], BF16)
    g.MKR_src = d("MKR_src", [32, TL], BF16); g.MKRC = d("MKRC", [32, CT], BF16)
    g.LX = d("LX", [256, TT], F32); g.LG = d("LG", [256, TT], F32)
    g.NAK_src = d("NAK_src", [TL, 256], BF16); g.NAKC = d("NAKC", [CT, 256], BF16)
    g.NAV_src = d("NAV_src", [TL, 260], BF16); g.NAVC = d("NAVC", [CT, 260], BF16)
    g.GV_src = d("GV_src", [TL, 130], BF16); g.GVC = d("GVC", [CT, 130], BF16)
    g.MV_src = d("MV_src", [TL, 260], BF16); g.MVC = d("MVC", [CT, 260], BF16)


def _stage_a(g, l):
    nc, kb, o, sb, ps, DK, bv = g.nc, g.kb, g.o, g.sb, g.ps, g.DK, g.bv
    _layer_dram(g)
    with contextlib.ExitStack() as ss:
        G = [sb(f"aG{w}", [128, 1024], F32, ss) for w in range(2)]
        SH = [sb(f"aSH{w}", [128, 1024], F32, ss) for w in range(2)]
        tmpA = sb("a_tmpA", [128, 1024], F32, ss); tmpB = sb("a_tmpB", [128, 1024], F32, ss)
        win = sb("a_win", [128, 8, 2208], BF16, ss)
        wuq = sb("a_wuq", [128, 2, 384], BF16, ss); wkn = sb("a_wkn", [128, 256], BF16, ss); wv = sb("a_wv", [128, 256], BF16, ss)
        cpk = sb("a_cpk", [128, NCOL], F32, ss); kn4 = sb("a_kn4", [128, 256], F32, ss)
        hT = sb("a_hT", [128, 8, TT], BF16, ss); hk = [Trk() for _ in range(NT)]
        xt = [sb(f"a_xt{i}", [128, 1024], F32, ss) for i in range(2)]
        hb = [sb(f"a_hb{i}", [128, 1024], BF16, ss) for i in range(2)]
        ssq = sb("a_ssq", [128, 2], F32, ss); srt1 = sb("a_srt1", [128, 2], F32, ss); rs1 = sb("a_rs1", [128, 2], F32, ss)
        rq = sb("a_rq", [128, 2, 512], F32, ss); rm = sb("a_rm", [128, 2, 512], F32, ss); rk = sb("a_rk", [32, 2, 512], F32, ss)
        sq = [sb(f"a_sq{i}", [128, 512], F32, ss) for i in range(2)]
        srt = sb("a_srt", [128, 512], F32, ss); rs = sb("a_rs", [128, 512], F32, ss)
        qn = sb("a_qn", [128, 512], F32, ss); t1 = sb("a_t1", [128, 512], F32, ss); t2 = sb("a_t2", [128, 512], F32, ss)
        ob = [sb(f"a_ob{i}", [128, 512], BF16, ss) for i in range(3)]
        of = [sb(f"a_of{i}", [128, 512], F32, ss) for i in range(2)]
        cqn = sb("a_cqn", [128, 2, 512], BF16, ss); ckvn = sb("a_ckvn", [128, 512], BF16, ss)
        sqk = sb("a_sqk", [128, 256], F32, ss); red4 = sb("a_red4", [128, 4, 1], F32, ss)
        srt4 = sb("a_srt4", [128, 4, 1], F32, ss); rs4 = sb("a_rs4", [128, 4, 1], F32, ss)
        nak = [sb(f"a_nak{i}", [128, 256], BF16, ss) for i in range(2)]
        vN = [sb(f"a_vN{i}", [128, 4, 65], BF16, ss) for i in range(2)]
        vG = [sb(f"a_vG{i}", [128, 2, 65], BF16, ss) for i in range(2)]
        vM = [sb(f"a_vM{i}", [128, 4, 65], BF16, ss) for i in range(2)]
        pu = [ps(f"a_pu{i}", [128, 512], F32, ss) for i in range(3)]
        pst = ps("a_pst", [128, 512], F32, ss); pr = ps("a_pr", [128, 512], F32, ss)
        pT = ps("a_pT", [128, 8, 128], BF16, ss); pvt = ps("a_pvt", [128, 512], F32, ss)

        BK = g.BK
        if SUB == 10: return
        stg = [sb(f"a_stg{i}", [128, 2208], F32, ss) for i in range(2)]
        for k in range(8):
            sg_ = stg[k % 2]
            o.dma(sg_[:, :], bv(f"w_in{l}", [[2208, 128], [1, 2208]], k * 128 * 2208), [BK], [sg_.k])
            o.cp(win[:, k, :], sg_[:, :], [sg_.k], [win.k], eng="pool")
        for c in range(2):
            sg_ = stg[c % 2]
            o.dma(sg_[:, 0:384], bv(f"wuq{l}", [[384, 128], [1, 384]], c * 128 * 384), [BK], [sg_.k])
            o.cp(wuq[:, c, :], sg_[:, 0:384], [sg_.k], [wuq.k], eng="pool")
        sg_ = stg[0]
        o.dma(sg_[:, 0:512], bv(f"wukv{l}", [[512, 128], [1, 512]]), [BK], [sg_.k])
        o.cp(wkn[:, :].rearrange("p (h d) -> p h d", d=64), sg_[:, 0:512].rearrange("p (h t) -> p h t", t=128)[:, :, 0:64], [sg_.k], [wkn.k], eng="pool")
        o.cp(wv[:, :].rearrange("p (h d) -> p h d", d=64), sg_[:, 0:512].rearrange("p (h t) -> p h t", t=128)[:, :, 64:128], [sg_.k], [wv.k], eng="pool")
        if SUB == 11: return
        o.dma(cpk[:, :], bv(f"cp{l}", [[NCOL, 128], [1, NCOL]]), [BK], [cpk.k])
        o.dma(kn4[:, :], bv(f"na_kn4{l}", [[0, 128], [1, 256]]), [BK], [kn4.k])
        def col(name, i=0, rows=128):
            c = CP[name][0] + i
            return cpk[0:rows, c:c + 1]
        if SUB == 12: return
        o.dma(tmpA[:, :], bv(f"norm1_g{l}", [[0, 128], [1, 1024]]), [BK], [tmpA.k])
        if SUB == 121: return
        for w in range(2):
            g.load_modvec(tmpB, l, w, 1)
            if SUB == 122: return
            o.stt(G[w][:, :], tmpB[:, :], 1.0, tmpA[:, :], ALU.add, ALU.mult, [tmpB.k, tmpA.k], [G[w].k])
            if SUB == 123: return
            g.load_modvec(SH[w], l, w, 0)
        if SUB == 13: return
        for i in range(2):
            o.ms(vN[i][:, :, :], 1.0, [vN[i].k]); o.ms(vG[i][:, :, :], 1.0, [vG[i].k]); o.ms(vM[i][:, :, :], 1.0, [vM[i].k])

        if SUB == 1: return
        XK = Trk()
        for t in range(NT):
            w = 0 if t < NTL else 1; i = t % 2
            o.dma(xt[i][:, :], g.iv('x', [[D, 128], [1, D]], t * 128 * D) if t < NTL else g.iv('ctx', [[D, 128], [1, D]], (t - NTL) * 128 * D), [XK], [xt[i].k])
            o.act(tmpA[:, :], xt[i][:, :], AF.Square, [xt[i].k], [tmpA.k, ssq.k], accum_out=ssq[:, i:i + 1])
            o.act(srt1[:, i:i + 1], ssq[:, i:i + 1], AF.Sqrt, [ssq.k], [srt1.k], scale=1.0 / D, bias=EPS)
            o.rcp(rs1[:, i:i + 1], srt1[:, i:i + 1], [srt1.k], [rs1.k])
            o.stt(tmpB[:, :], xt[i][:, :], rs1[:, i:i + 1], G[w][:, :], ALU.mult, ALU.mult, [xt[i].k, rs1.k, G[w].k], [tmpB.k])
            o.tt(hb[i][:, :], tmpB[:, :], SH[w][:, :], ALU.add, [tmpB.k, SH[w].k], [hb[i].k])
            for k in range(8):
                o.tr(pT[:, k, :], hb[i][:, k * 128:(k + 1) * 128], g.ident_b[:, :], [hb[i].k, g.ident_b.k], [pT.k])
            o.cp(hT[:, :, t * 128:(t + 1) * 128], pT[:, :, :], [pT.k], [hk[t]])

        if SUB == 2: return
        def hks(t0, n):
            return [hk[t] for t in range(t0 // 128, (t0 + n) // 128)]
        def proj(pt, col0, M, t0, n):
            for k in range(8):
                o.mm(pt[0:M, 0:n], win[:, k, col0:col0 + M], hT[:, k, t0:t0 + n], k == 0, k == 7, [win.k] + hks(t0, n), [pt.k])
        def headnorm(pu_, M, n, ones_ap, ones_k, scale, gcol, out_t, sqi=0):
            o.act(sq[sqi][0:M, 0:n], pu_[0:M, 0:n], AF.Square, [pu_.k], [sq[sqi].k])
            o.mm(pst[0:M, 0:n], ones_ap, sq[sqi][0:M, 0:n], True, True, [ones_k, sq[sqi].k], [pst.k])
            o.act(srt[0:M, 0:n], pst[0:M, 0:n], AF.Sqrt, [pst.k], [srt.k], scale=scale, bias=EPS)
            o.rcp(rs[0:M, 0:n], srt[0:M, 0:n], [srt.k], [rs.k])
            o.stt(out_t[0:M, 0:n], pu_[0:M, 0:n], gcol, rs[0:M, 0:n], ALU.mult, ALU.mult, [pu_.k, rs.k, cpk.k], [out_t.k])
        def rope(qn_, M, n, prot, tab, out_t):
            o.mm(pr[0:M, 0:n], prot[0:M, 0:M], qn_[0:M, 0:n], True, True, [prot.k, qn_.k], [pr.k])
            o.tt(t1[0:M, 0:n], qn_[0:M, 0:n], tab[0:M, 0, 0:n], ALU.mult, [qn_.k, tab.k], [t1.k])
            o.tt(t2[0:M, 0:n], pr[0:M, 0:n], tab[0:M, 1, 0:n], ALU.mult, [pr.k, tab.k], [t2.k])
            o.tt(out_t[0:M, 0:n], t1[0:M, 0:n], t2[0:M, 0:n], ALU.add, [t1.k, t2.k], [out_t.k])
        obi = [0]
        def nob():
            obi[0] = (obi[0] + 1) % 3
            return ob[obi[0]]

        for (t0, n) in TCH:
            lat = t0 < TL
            if lat:
                o.dma(rq[:, :, 0:n], g.iv("ropeq", [[TL, 128], [128 * TL, 2], [1, n]], t0), [], [rq.k])
                o.dma(rm[:, :, 0:n], g.iv("ropem", [[TL, 128], [128 * TL, 2], [1, n]], t0), [], [rm.k])
                o.dma(rk[:, :, 0:n], g.iv("ropek", [[TL, 32], [32 * TL, 2], [1, n]], t0), [], [rk.k])
            c0 = t0 if lat else t0 - TL
            for c in range(2):
                proj(pu[c], c * 128, 128, t0, n)
                B = nob()
                headnorm(pu[c], 128, n, g.ones64[:, :], g.ones64.k, 1.0 / 64, col("na_qn2"), B)
                o.dma(g.NQ[c * 128:(c + 1) * 128, t0:t0 + n], B[:, 0:n], [B.k], [DK(g.NQ)])
            for c in range(3):
                proj(pu[c], 768 + c * 128, 128, t0, n)
                B = nob()
                gname = "gqa_qn2" if c < 2 else "gqa_kn2"
                if lat:
                    headnorm(pu[c], 128, n, g.ones64[:, :], g.ones64.k, 1.0 / 64, col(gname), qn)
                    rope(qn, 128, n, g.prot64, rq, B)
                else:
                    headnorm(pu[c], 128, n, g.ones64[:, :], g.ones64.k, 1.0 / 64, col(gname), B)
                if c < 2:
                    o.dma(g.GQ[c * 128:(c + 1) * 128, t0:t0 + n], B[:, 0:n], [B.k], [DK(g.GQ)])
                elif lat:
                    o.dma(g.GK_src[:, c0:c0 + n], B[:, 0:n], [B.k], [DK(g.GK_src)])
                else:
                    o.dma(g.GKC[:, c0:c0 + n], B[:, 0:n], [B.k], [DK(g.GKC)])
            for c in range(2):
                proj(pu[c], 1280 + c * 128, 128, t0, n)
                o.act(sq[c][:, 0:n], pu[c][:, 0:n], AF.Square, [pu[c].k], [sq[c].k])
            for c in range(2):
                o.mm(pst[:, 0:n], g.ones128[:, :], sq[c][:, 0:n], c == 0, c == 1, [g.ones128.k, sq[c].k], [pst.k])
            o.act(srt[:, 0:n], pst[:, 0:n], AF.Sqrt, [pst.k], [srt.k], scale=1.0 / 256, bias=EPS)
            o.rcp(rs[:, 0:n], srt[:, 0:n], [srt.k], [rs.k])
            for c in range(2):
                o.stt(cqn[:, c, 0:n], pu[c][:, 0:n], col("mla_qa_g", c), rs[:, 0:n], ALU.mult, ALU.mult, [pu[c].k, rs.k, cpk.k], [cqn.k])
            for h in range(4):
                for c in range(2):
                    o.mm(pu[2][0:96, 0:n], wuq[:, c, 96 * h:96 * h + 96], cqn[:, c, 0:n], c == 0, c == 1, [wuq.k, cqn.k], [pu[2].k])
                B = nob()
                if lat:
                    headnorm(pu[2], 96, n, g.blk96[0:96, 0:96], g.blk96.k, col("sc96", 0, 96), col("mla_qn", 0, 96), qn)
                    rope(qn, 96, n, g.prot96, rm, B)
                else:
                    headnorm(pu[2], 96, n, g.blk96[0:96, 0:96], g.blk96.k, col("sc96", 0, 96), col("mla_qn", 0, 96), B)
                o.dma(g.MQ[h * 96:(h + 1) * 96, t0:t0 + n], B[0:96, 0:n], [B.k], [DK(g.MQ)])
            proj(pu[0], 1536, 128, t0, n)
            o.act(sq[0][:, 0:n], pu[0][:, 0:n], AF.Square, [pu[0].k], [sq[0].k])
            o.mm(pst[:, 0:n], g.ones128[:, :], sq[0][:, 0:n], True, True, [g.ones128.k, sq[0].k], [pst.k])
            o.act(srt[:, 0:n], pst[:, 0:n], AF.Sqrt, [pst.k], [srt.k], scale=1.0 / 128, bias=EPS)
            o.rcp(rs[:, 0:n], srt[:, 0:n], [srt.k], [rs.k])
            o.stt(ckvn[:, 0:n], pu[0][:, 0:n], col("mla_kva_g"), rs[:, 0:n], ALU.mult, ALU.mult, [pu[0].k, rs.k, cpk.k], [ckvn.k])
            for p in range(2):
                o.mm(pu[1][:, 0:n], wkn[:, p * 128:(p + 1) * 128], ckvn[:, 0:n], True, True, [wkn.k, ckvn.k], [pu[1].k])
                B = nob()
                headnorm(pu[1], 128, n, g.ones64[:, :], g.ones64.k, 1.0 / 64, col("mla_knn2"), B)
                if lat:
                    o.dma(g.MKN_src[p * 128:(p + 1) * 128, c0:c0 + n], B[:, 0:n], [B.k], [DK(g.MKN_src)])
                else:
                    o.dma(g.MKNC[p * 128:(p + 1) * 128, c0:c0 + n], B[:, 0:n], [B.k], [DK(g.MKNC)])
            for j in range(n // 128):
                V = vM[j % 2]
                o.mm(pvt[:, 0:256], ckvn[:, j * 128:(j + 1) * 128], wv[:, :], True, True, [ckvn.k, wv.k], [pvt.k])
                o.cp(V[:, :, 0:64], pvt[:, 0:256].rearrange("p (h d) -> p h d", d=64), [pvt.k], [V.k])
                r0 = c0 + j * 128
                dst = g.MV_src if lat else g.MVC
                o.dma(dst[r0:r0 + 128, :], V[:, :, :].rearrange("p h d -> p (h d)"), [V.k], [DK(dst)])
            proj(pu[2], 1664, 32, t0, n)
            B = nob()
            if lat:
                headnorm(pu[2], 32, n, g.ones128[0:32, 0:32], g.ones128.k, 1.0 / 32, col("mla_knr", 0, 32), qn)
                rope(qn, 32, n, g.prot32, rk, B)
                o.dma(g.MKR_src[:, c0:c0 + n], B[0:32, 0:n], [B.k], [DK(g.MKR_src)])
            else:
                headnorm(pu[2], 32, n, g.ones128[0:32, 0:32], g.ones128.k, 1.0 / 32, col("mla_knr", 0, 32), B)
                o.dma(g.MKRC[:, c0:c0 + n], B[0:32, 0:n], [B.k], [DK(g.MKRC)])
            for c in range(4):
                proj(pu[c % 3], 1696 + c * 128, 128, t0, n)
                Fo = of[c % 2]
                o.cp(Fo[:, 0:n], pu[c % 3][:, 0:n], [pu[c % 3].k], [Fo.k], eng="act" if False else "dve")
                dst = g.LX if c < 2 else g.LG
                cc_ = c % 2
                o.dma(dst[cc_ * 128:(cc_ + 1) * 128, t0:t0 + n], Fo[:, 0:n], [Fo.k], [DK(dst)])

        if SUB == 3: return
        for t in range(NT):
            lat = t < NTL; i = t % 2
            r0 = t * 128 if lat else (t - NTL) * 128
            for k in range(8):
                o.mm(pvt[:, 0:512], hT[:, k, t * 128:(t + 1) * 128], win[:, k, 256:768], k == 0, k == 7, [hk[t], win.k], [pvt.k])
            o.act(sqk[:, :], pvt[:, 0:256], AF.Square, [pvt.k], [sqk.k])
            o.red(red4[:, :, 0], sqk[:, :].rearrange("p (h d) -> p h d", d=64), ALU.add, [sqk.k], [red4.k])
            o.act(srt4[:, :, :], red4[:, :, :], AF.Sqrt, [red4.k], [srt4.k], scale=1.0 / 64, bias=EPS)
            o.rcp(rs4[:, :, :], srt4[:, :, :], [srt4.k], [rs4.k])
            o.tt(sqk[:, :].rearrange("p (h d) -> p h d", d=64), pvt[:, 0:256].rearrange("p (h d) -> p h d", d=64),
                 rs4[:, :, 0:1].to_broadcast([128, 4, 64]), ALU.mult, [pvt.k, rs4.k], [sqk.k])
            o.tt(nak[i][:, :], sqk[:, :], kn4[:, :], ALU.mult, [sqk.k, kn4.k], [nak[i].k])
            dst = g.NAK_src if lat else g.NAKC
            o.dma(dst[r0:r0 + 128, :], nak[i][:, :], [nak[i].k], [DK(dst)])
            o.cp(vN[i][:, :, 0:64], pvt[:, 256:512].rearrange("p (h d) -> p h d", d=64), [pvt.k], [vN[i].k])
            dst = g.NAV_src if lat else g.NAVC
            o.dma(dst[r0:r0 + 128, :], vN[i][:, :, :].rearrange("p h d -> p (h d)"), [vN[i].k], [DK(dst)])
            for k in range(8):
                o.mm(pu[0][:, 0:128], hT[:, k, t * 128:(t + 1) * 128], win[:, k, 1152:1280], k == 0, k == 7, [hk[t], win.k], [pu[0].k])
            o.cp(vG[i][:, :, 0:64], pu[0][:, 0:128].rearrange("p (h d) -> p h d", d=64), [pu[0].k], [vG[i].k])
            dst = g.GV_src if lat else g.GVC
            o.dma(dst[r0:r0 + 128, :], vG[i][:, :, :].rearrange("p h d -> p (h d)"), [vG[i].k], [DK(dst)])


_CACHE = {}
def run(nc, maps):
    res = run_bass_kernel_spmd(nc, maps, core_ids=list(range(NCORE)))
    return res.results


def host_p0(inp, nlayers=L):
    c = inp["c"].reshape(D); cc = inp["c_ctx"].reshape(D)
    maps = []
    for j in range(NCORE):
        pk = Packer()
        pk.add("cT2", np.concatenate([c.reshape(8, 128).T, cc.reshape(8, 128).T], axis=1))
        pk.add("adaw", inp["ada_w"][:nlayers, :, j * 768:(j + 1) * 768])
        pk.add("adab", inp["ada_b"][:nlayers, j * 768:(j + 1) * 768])
        arr, irows = pk.finish_single()
        maps.append({"inp": arr})
    nc = build(emit_p0, pk.items, irows, nlayers=nlayers)
    res = run(nc, maps)
    mod = np.concatenate([np.asarray(r["mod"]).reshape(2, nlayers, 768) for r in res], axis=2)
    return mod


def rope_items(pk, j):
    c64, s64 = _rope_tables(j * TL, TL, 64)
    pk.add("ropeq", np.stack([np.concatenate([c64, c64]), np.concatenate([s64, s64])]))
    c32, s32 = _rope_tables(j * TL, TL, 32)
    cm = np.ones((128, TL), np.float32); sm = np.zeros((128, TL), np.float32)
    cm[64:96] = c32; sm[64:96] = s32
    pk.add("ropem", np.stack([cm, sm]))
    pk.add("ropek", np.stack([c32, s32]))


def host_p1(inp, l, x, xc, mod):
    maps = []
    for j in range(NCORE):
        pk = Packer()
        add_consts(pk)
        pk.add("w_in", inp["w_in"][l]); pk.add("wuq", inp["mla_wuq"][l]); pk.add("wukv", inp["mla_wukv"][l])
        pk.add("norm1_g", inp["norm1_g"][l]); pk.add("na_kn4", np.tile(inp["na_kn"][l], 4)); pk.add("cp", make_cp(inp, l))
        pk.add("x", x[j * TL:(j + 1) * TL]); pk.add("ctx", xc)
        pk.add("modv", mod[:, l, :].reshape(2, 6, 1024))
        rope_items(pk, j)
        arr, irows = pk.finish_single()
        maps.append({"inp": arr})
    if "p1" not in _CACHE: _CACHE["p1"] = build(emit_p1, pk.items, irows)
    return run(_CACHE["p1"], maps)


def na_var(lr):
    return 0 if 4 <= lr <= 28 else (1 + lr if lr < 4 else 5 + (lr - 29))


def emit_p2(g):
    nc, kb, o, sb, ps, DK, iv = g.nc, g.kb, g.o, g.sb, g.ps, g.DK, g.iv
    d = g.dram
    I = lambda n, s, dt: d(n, s, dt, "ExternalInput")
    gq = I("gq", [256, TT], BF16); gk = I("gk", [128, S + CT], BF16); gv = I("gv", [S + CT, 130], BF16)
    mq = I("mq", [384, TT], BF16); mk = I("mk", [384, S + CT], BF16); mv = I("mv", [S + CT, 260], BF16)
    nq = I("nq", [256, TT], BF16); nkt = I("nkt", [32, 256, 512], BF16); nv = I("nv", [32, 512, 260], BF16)
    nkc = I("nkc", [256, CT], BF16); nvc = I("nvc", [CT, 260], BF16)
    eb = I("ebias", [8, 4, 512, 64], F32)
    lxp = I("lxp", [2, 64, 259 + S + 3], F32)
    OT = g.out("OT", [768, TT], F32)
    LY = g.out("LY", [2, 64, CT + S], F32)
    NK = (S + CT) // 128
    ones = sb("p2_ones", [128, 64], F32)
    o.ms(ones[:, :], 1.0, [ones.k])
    TK = Trk()

    ps_s = [ps(f"p2_s{i}", [128, 512], F32) for i in range(3)]
    ps_o = [ps(f"p2_o{i}", [128, 512], F32) for i in range(2)]
    ps_b = ps("p2_b", [128, 512], F32)
    ps_g = [ps(f"p2_g{i}", [128, 512], F32) for i in range(2)]
    pbuf = [sb(f"p2_p{i}", [128, 512], BF16) for i in range(3)]
    rsb = sb("p2_rs", [128, 512], F32); osb = sb("p2_osb", [64, 512], F32); onb = [sb(f"p2_on{i}", [64, 512], F32) for i in range(2)]
    cnt = [0]

    def finalize(po, n, row0, col0):
        o.rcp(rsb[64:65, 0:n], po[64:65, 0:n], [po.k], [rsb.k])
        o.mm(ps_b[0:64, 0:n], ones[64:65, 0:64], rsb[64:65, 0:n], True, True, [ones.k, rsb.k], [ps_b.k])
        o.cp(osb[:, 0:n], po[0:64, 0:n], [po.k], [osb.k], eng="act" if False else "dve")
        B = onb[cnt[0] % 2]; cnt[0] += 1
        o.tt(B[:, 0:n], osb[:, 0:n], ps_b[0:64, 0:n], ALU.mult, [osb.k, ps_b.k], [B.k])
        o.dma(OT[row0:row0 + 64, col0:col0 + n], B[:, 0:n], [B.k], [DK(OT)])

    def dense_head(QT, dk, KT, VA, vsl, scale, row0, kts_lat, kts_ctx):
        it = 0
        for (q0, n, kts) in [(c * 512, 512, kts_lat) for c in range(4)] + [(TL, CT, kts_ctx)]:
            po = ps_o[(q0 // 512) % 2]
            for ki, kt in enumerate(kts):
                pS = ps_s[it % 3]; P = pbuf[it % 3]; it += 1
                o.mm(pS[:, 0:n], KT[0:dk, kt * 128:(kt + 1) * 128], QT[0:dk, q0:q0 + n], True, True, [KT.k, QT.k], [pS.k])
                o.act(P[:, 0:n], pS[:, 0:n], AF.Exp, [pS.k], [P.k], scale=scale)
                o.mm(po[0:65, 0:n], vsl(kt), P[:, 0:n], ki == 0, ki == len(kts) - 1, [VA.k, P.k], [po.k])
            finalize(po, n, row0, q0)

    with contextlib.ExitStack() as ss:
        KT = sb("g_KT", [64, NK * 128], BF16, ss); VA = sb("g_VA", [128, NK, 130], BF16, ss)
        QT = [sb(f"g_QT{i}", [64, TT], BF16, ss) for i in range(2)]
        o.dma(VA[:, :, :], dap(gv, 0, [[130, 128], [128 * 130, NK], [1, 130]]), [], [VA.k])
        for h in range(4):
            kvh = h // 2
            if h % 2 == 0:
                o.dma(KT[:, :], gk[kvh * 64:(kvh + 1) * 64, :], [], [KT.k])
            Q = QT[h % 2]
            o.dma(Q[:, :], gq[h * 64:(h + 1) * 64, :], [], [Q.k])
            dense_head(Q, 64, KT, VA, lambda kt, kvh=kvh: VA[:, kt, kvh * 65:(kvh + 1) * 65], 0.125, 256 + h * 64,
                       list(range(NK)), [NK - 2, NK - 1])
    with contextlib.ExitStack() as ss:
        KT = sb("m_KT", [96, NK * 128], BF16, ss); VA = sb("m_VA", [128, NK, 260], BF16, ss)
        QT = [sb(f"m_QT{i}", [96, TT], BF16, ss) for i in range(2)]
        o.dma(VA[:, :, :], dap(mv, 0, [[260, 128], [128 * 260, NK], [1, 260]]), [], [VA.k])
        for h in range(4):
            o.dma(KT[:, :], mk[h * 96:(h + 1) * 96, :], [], [KT.k])
            Q = QT[h % 2]
            o.dma(Q[:, :], mq[h * 96:(h + 1) * 96, :], [], [Q.k])
            dense_head(Q, 96, KT, VA, lambda kt, h=h: VA[:, kt, h * 65:(h + 1) * 65], 96 ** -0.5, 512 + h * 64,
                       list(range(NK)), [NK - 2, NK - 1])
    with contextlib.ExitStack() as ss:
        QT = sb("n_QT", [64, 4, TT], BF16, ss)
        KC = sb("n_KC", [64, 4, CT], BF16, ss); VC = sb("n_VC", [128, 2, 260], BF16, ss)
        E = sb("n_E", [128, 8, 4, 4, 64], BF16, ss); Ef = sb("n_Ef", [128, 4, 4, 64], F32, ss)
        KR = [sb(f"n_KR{i}", [64, 4, 512], BF16, ss) for i in range(2)]
        VR = [sb(f"n_VR{i}", [128, 4, 260], BF16, ss) for i in range(2)]
        Pn = [sb(f"n_P{i}", [128, 384], BF16, ss) for i in range(2)]; Pf = sb("n_Pf", [128, 384], F32, ss)
        for h in range(4):
            o.dma(QT[:, h, :], nq[h * 64:(h + 1) * 64, :], [], [QT.k])
            o.dma(KC[:, h, :], nkc[h * 64:(h + 1) * 64, :], [], [KC.k])
        o.dma(VC[:, :, :], dap(nvc, 0, [[260, 128], [128 * 260, 2], [1, 260]]), [], [VC.k])
        for v in range(8):
            for h in range(4):
                o.dma(Ef[:, h, :, :], dap(eb, (v * 4 + h) * 512 * 64, [[64, 128], [128 * 64, 4], [1, 64]]), [], [Ef.k])
            o.act(E[:, v, :, :, :], Ef[:, :, :, :], AF.Exp, [Ef.k], [E.k])
        it = 0
        for h in range(4):
            for lr in range(32):
                if h == 0 or True:
                    pass
            pass
        for rg in range(4):
            for h in range(4):
                pass
        for rg in range(4):
            for lr8 in range(8):
                lr = rg * 8 + lr8
                K_ = KR[lr % 2]; V_ = VR[lr % 2]
                o.dma(K_[:, :, :], dap(nkt, lr * 256 * 512, [[512, 64], [64 * 512, 4], [1, 512]]), [], [K_.k])
                o.dma(V_[:, :, :], dap(nv, lr * 512 * 260, [[260, 128], [128 * 260, 4], [1, 260]]), [], [V_.k])
                for h in range(4):
                    po = (ps_o + ps_g)[h]
                    pS = ps_s[it % 3]; P = Pn[it % 2]; it += 1
                    q = QT[:, h, lr * 64:(lr + 1) * 64]
                    for kt in range(4):
                        o.mm(pS[:, kt * 64:(kt + 1) * 64], K_[:, h, kt * 128:(kt + 1) * 128], q, True, True, [K_.k, QT.k], [pS.k])
                    for kt in range(2):
                        o.mm(pS[:, 256 + kt * 64:256 + (kt + 1) * 64], KC[:, h, kt * 128:(kt + 1) * 128], q, True, True, [KC.k, QT.k], [pS.k])
                    o.act(Pf[:, 0:256], pS[:, 0:256], AF.Exp, [pS.k], [Pf.k], scale=0.125)
                    o.act(P[:, 256:384], pS[:, 256:384], AF.Exp, [pS.k], [P.k], scale=0.125)
                    o.tt(P[:, 0:256], Pf[:, 0:256], E[:, na_var(lr), h, :, :].rearrange("p a b -> p (a b)"), ALU.mult, [Pf.k, E.k], [P.k])
                    for kt in range(6):
                        lhs = V_[:, kt, h * 65:(h + 1) * 65] if kt < 4 else VC[:, kt - 4, h * 65:(h + 1) * 65]
                        o.mm(po[0:65, lr8 * 64:(lr8 + 1) * 64], lhs, P[:, kt * 64:(kt + 1) * 64], kt == 0, kt == 5, [V_.k, VC.k, P.k], [po.k])
            for h in range(4):
                finalize((ps_o + ps_g)[h], 512, h * 64, rg * 512)
        for h in range(4):
            po = (ps_o + ps_g)[h]
            pS = ps_s[it % 3]; it += 1
            P2 = pbuf[h % 3]
            for kt in range(2):
                o.mm(pS[:, kt * 256:(kt + 1) * 256], KC[:, h, kt * 128:(kt + 1) * 128], QT[:, h, TL:TT], True, True, [KC.k, QT.k], [pS.k])
            o.act(P2[:, 0:512], pS[:, 0:512], AF.Exp, [pS.k], [P2.k], scale=0.125)
            for kt in range(2):
                o.mm(po[0:65, 0:256], VC[:, kt, h * 65:(h + 1) * 65], P2[:, kt * 256:(kt + 1) * 256], kt == 0, kt == 1, [VC.k, P2.k], [po.k])
            finalize(po, 256, h * 64, TL)
    with contextlib.ExitStack() as ss:
        W = sb("l_W", [64, 2, 2, 64], F32, ss)
        cpl = sb("l_cp", [64, 16], F32, ss)
        cneg = sb("l_cneg", [64, 2], F32, ss); tmpc = sb("l_tmpc", [64, 2], F32, ss)
        xp = sb("l_xp", [64, 2051], F32, ss); xc_ = sb("l_xc", [64, 2048], F32, ss)
        ra = sb("l_ra", [64, 2048], F32, ss); ri = sb("l_ri", [64, 2048], F32, ss)
        a2 = sb("l_a2", [64, 2048], F32, ss); hh = sb("l_h", [64, 2048], F32, ss)
        hprev = sb("l_hp", [64, 1], F32, ss)
        o.dma(W[:, :, :, :], iv("lru_w", [[64, 64], [64 * 64, 4], [1, 64]]).rearrange("k (d t) m -> k d t m", t=2), [], [W.k])
        o.dma(cpl[:, :], iv("lru_cp", [[16, 64], [1, 16]]), [], [cpl.k])
        o.act(tmpc[:, :], cpl[:, 9:11], AF.Exp, [cpl.k], [tmpc.k], scale=-1.0)
        o.act(tmpc[:, :], tmpc[:, :], AF.Ln, [tmpc.k], [tmpc.k], bias=1.0)
        o.ts(cneg[:, :], tmpc[:, :], -8.0, None, ALU.mult, ALU.bypass, [tmpc.k], [cneg.k])
        for dr in range(2):
            first = True
            for (p0, n, o0) in [(0, CT, 0)] + [(259 + c * 2048, 2048, CT + c * 2048) for c in range(8)]:
                o.dma(xp[:, 0:n + 3], dap(lxp, dr * 64 * (262 + S) + p0, [[262 + S, 64], [1, n + 3]]), [], [xp.k])
                for k in range(4):
                    off = k if dr == 0 else 3 - k
                    if k == 0:
                        o.ts(xc_[:, 0:n], xp[:, off:off + n], cpl[:, 0:1], cpl[:, 4:5], ALU.mult, ALU.add, [xp.k, cpl.k], [xc_.k])
                    else:
                        o.stt(xc_[:, 0:n], xp[:, off:off + n], cpl[:, k:k + 1], xc_[:, 0:n], ALU.mult, ALU.add, [xp.k, cpl.k, xc_.k], [xc_.k])
                for c in range(0, n, 512):
                    cn = min(512, n - c)
                    for ty, dst in ((0, ra), (1, ri)):
                        pg = ps_g[ty]
                        o.mm(pg[0:64, 0:cn], W[:, dr, ty, :], xc_[:, c:c + cn], True, True, [W.k, xc_.k], [pg.k])
                        bcol = cpl[:, 5 + 2 * ty + dr:6 + 2 * ty + dr]
                        o.act(dst[:, c:c + cn], pg[0:64, 0:cn], AF.Sigmoid, [pg.k, cpl.k], [dst.k], bias=bcol)
                o.act(ra[:, 0:n], ra[:, 0:n], AF.Exp, [ra.k, cneg.k], [ra.k], scale=cneg[:, dr:dr + 1])
                o.tt(a2[:, 0:n], ra[:, 0:n], ra[:, 0:n], ALU.mult, [ra.k], [a2.k])
                o.act(a2[:, 0:n], a2[:, 0:n], AF.Sqrt, [a2.k], [a2.k], scale=-1.0, bias=1.0)
                o.tt(ri[:, 0:n], ri[:, 0:n], xc_[:, 0:n], ALU.mult, [ri.k, xc_.k], [ri.k])
                o.tt(ri[:, 0:n], ri[:, 0:n], a2[:, 0:n], ALU.mult, [ri.k, a2.k], [ri.k])
                init = 0.0 if first else hprev[:, 0:1]
                kb.op("dve", lambda e, n=n, init=init: e.tensor_tensor_scan(out=hh[:, 0:n], data0=ra[:, 0:n], data1=ri[:, 0:n],
                                                                            initial=init, op0=ALU.mult, op1=ALU.add),
                      [ra.k, ri.k, hprev.k], [hh.k])
                o.cp(hprev[:, 0:1], hh[:, n - 1:n], [hh.k], [hprev.k])
                o.dma(LY[dr, :, o0:o0 + n], hh[:, 0:n], [hh.k], [DK(LY)])
                first = False


def _ebias_table(rpb, j):
    out = np.full((8, 4, 8, 64, 64), -30000.0, np.float32)
    c = np.arange(64); c0 = np.clip(c - 8, 0, 48)
    kc = np.arange(64)
    valid = (kc[:, None] >= c0[None, :]) & (kc[:, None] < c0[None, :] + 16)
    relc = np.clip(kc[:, None] - c[None, :] + 15, 0, 30)
    lrs = [4, 0, 1, 2, 3, 29, 30, 31]
    for v, lr in enumerate(lrs):
        r = 32 * j + lr; r0 = min(max(r - 4, 0), 248); dl = r - r0
        for i in range(8):
            dr_ = i - dl + 7
            tab = rpb[:, dr_, :][:, relc]
            out[v, :, i] = np.where(valid[None], tab, np.float32(-30000.0))
    return out.reshape(8, 4, 512, 64)


def host_p2(inp, l, p1):
    f = lambda n: [np.asarray(r[n]) for r in p1]
    cat = lambda n, ax: np.concatenate(f(n), axis=ax)
    gk_all = np.concatenate([cat("GK_src", 1), p1[0]["GKC"]], axis=1)
    gv_all = np.concatenate([cat("GV_src", 0), p1[0]["GVC"]], axis=0)
    mkn = np.concatenate([cat("MKN_src", 1), p1[0]["MKNC"]], axis=1)
    mkr = np.concatenate([cat("MKR_src", 1), p1[0]["MKRC"]], axis=1)
    mk_all = np.concatenate([np.concatenate([mkn[h * 64:(h + 1) * 64], mkr], axis=0) for h in range(4)], axis=0)
    mv_all = np.concatenate([cat("MV_src", 0), p1[0]["MVC"]], axis=0)
    nak_all = cat("NAK_src", 0); nav_all = cat("NAV_src", 0)
    lx_l = cat("LX", 1)
    LX = [np.asarray(r["LX"]) for r in p1]
    lx_lat = np.concatenate([a[:, :TL] for a in LX], axis=1)
    lx_ctx = LX[0][:, TL:]
    maps = []
    for j in range(NCORE):
        m = {}
        m["gq"] = p1[j]["GQ"]; m["gk"] = gk_all; m["gv"] = gv_all
        m["mq"] = p1[j]["MQ"]; m["mk"] = mk_all; m["mv"] = mv_all
        m["nq"] = p1[j]["NQ"]
        nkt = np.empty((32, 256, 512), nak_all.dtype); nvv = np.empty((32, 512, 260), nav_all.dtype)
        for lr in range(32):
            r = 32 * j + lr; r0 = min(max(r - 4, 0), 248)
            nkt[lr] = nak_all[r0 * 64:r0 * 64 + 512].T
            nvv[lr] = nav_all[r0 * 64:r0 * 64 + 512]
        m["nkt"] = nkt; m["nv"] = nvv
        m["nkc"] = np.ascontiguousarray(np.asarray(p1[0]["NAKC"]).T); m["nvc"] = p1[0]["NAVC"]
        m["ebias"] = _ebias_table(np.asarray(inp["na_rpb"][l], np.float32), j)
        gb = j % 4
        def padded(a):
            return np.concatenate([np.zeros((64, 1), np.float32), a, np.zeros((64, 2), np.float32)], axis=1)
        pc = padded(lx_ctx[gb * 64:(gb + 1) * 64]); pl = padded(lx_lat[gb * 64:(gb + 1) * 64])
        m["lxp"] = np.ascontiguousarray(np.stack([np.concatenate([pc, pl], axis=1),
                                                  np.concatenate([pc[:, ::-1], pl[:, ::-1]], axis=1)]))
        pk = Packer()
        w = np.stack([(inp["lru_wa"] if t == 0 else inp["lru_wi"])[l][d_][gb] for d_ in range(2) for t in range(2)])
        pk.add("lru_w", w)
        cpl = np.zeros((64, 16), np.float32)
        sl = slice(gb * 64, (gb + 1) * 64)
        cpl[:, 0:4] = inp["lru_conv_w"][l].reshape(4, 256)[:, sl].T
        cpl[:, 4] = inp["lru_conv_b"][l][sl]
        cpl[:, 5:7] = inp["lru_ba"][l][:, sl].T; cpl[:, 7:9] = inp["lru_bi"][l][:, sl].T; cpl[:, 9:11] = inp["lru_lam"][l][:, sl].T
        pk.add("lru_cp", cpl)
        arr, irows = pk.finish_single()
        m["inp"] = arr
        maps.append({k: np.ascontiguousarray(v) for k, v in m.items()})
    if "p2" not in _CACHE: _CACHE["p2"] = build(emit_p2, pk.items, irows)
    return run(_CACHE["p2"], maps)


def emit_p3(g):
    nc, kb, o, sb, ps, DK, iv = g.nc, g.kb, g.o, g.sb, g.ps, g.DK, g.iv
    d = g.dram
    I = lambda n, s, dt: d(n, s, dt, "ExternalInput")
    OTi = I("ot", [768, TT], F32); LYi = I("ly", [2, 256, TT], F32); LGi = I("lg", [256, TT], F32)
    X1 = g.out("X1", [TT, D], F32); H2 = g.out("H2", [TT, D], BF16); RT = g.out("RT", [TT, 8], F32)
    load_consts(g, ["ident", "ones128"])
    wout = sb("wout", [128, 8, 1024], BF16); stg = [sb(f"stg{i}", [128, 1024], F32) for i in range(2)]
    for k in range(8):
        s_ = stg[k % 2]
        o.dma(s_[:, :], iv("w_out", [[1024, 128], [1, 1024]], k * 128 * 1024), [], [s_.k])
        o.cp(wout[:, k, :], s_[:, :], [s_.k], [wout.k], eng="pool")
    rw = sb("rw", [128, 8, 32], F32); rb = sb("rb", [128, 32], F32); cpk = sb("cpk", [128, NCOL], F32)
    o.dma(rw[:, :, :], iv("router_w", [[32, 128], [128 * 32, 8], [1, 32]]), [], [rw.k])
    o.dma(rb[:, :], iv("router_b", [[0, 128], [1, 32]]), [], [rb.k])
    o.dma(cpk[:, :], iv("cp", [[NCOL, 128], [1, NCOL]]), [], [cpk.k])
    GT1 = [sb(f"GT1{w}", [128, 1024], F32) for w in range(2)]; G2 = [sb(f"G2{w}", [128, 1024], F32) for w in range(2)]
    SH2 = [sb(f"SH2{w}", [128, 1024], F32) for w in range(2)]
    tA = sb("tA", [128, 1024], F32); tB = sb("tB", [128, 1024], F32)
    mv_ = lambda w, v: iv("modv", [[0, 128], [1, 1024]], (w * 6 + v) * 1024)
    o.dma(tA[:, :], iv("norm2_g", [[0, 128], [1, 1024]]), [], [tA.k])
    for w in range(2):
        o.dma(GT1[w][:, :], mv_(w, 2), [], [GT1[w].k])
        o.dma(tB[:, :], mv_(w, 4), [], [tB.k])
        o.stt(G2[w][:, :], tB[:, :], 1.0, tA[:, :], ALU.add, ALU.mult, [tB.k, tA.k], [G2[w].k])
        o.dma(SH2[w][:, :], mv_(w, 3), [], [SH2[w].k])
    O_ = sb("O_", [128, 8, 512], F32); yf = sb("yf", [128, 512], F32); yb = sb("yb", [128, 512], F32); lgt = sb("lgt", [128, 512], F32)
    sq = [sb(f"sq{i}", [128, 512], F32) for i in range(2)]; srt = sb("srt", [128, 512], F32); rs = sb("rs", [128, 512], F32)
    yn = sb("yn", [128, 8, 512], BF16)
    xt = [sb(f"xt{i}", [128, 1024], F32) for i in range(2)]; x1 = [sb(f"x1{i}", [128, 1024], F32) for i in range(2)]
    h2 = [sb(f"h2{i}", [128, 1024], F32) for i in range(2)]; h2b = [sb(f"h2b{i}", [128, 1024], BF16) for i in range(2)]
    h2T = sb("h2T", [128, 8, 128], F32)
    ssq = sb("ssq", [128, 2], F32); srt1 = sb("srt1", [128, 2], F32); rs1 = sb("rs1", [128, 2], F32)
    lg32 = sb("lg32", [128, 32], F32); v8 = sb("v8", [128, 8], F32); i8 = sb("i8", [128, 8], U32)
    nv0 = sb("nv0", [128, 1], F32); se = sb("se", [128, 1], F32); rse = sb("rse", [128, 1], F32); e4 = sb("e4", [128, 4], F32)
    rtt = [sb(f"rtt{i}", [128, 8], F32) for i in range(2)]
    pst = ps("pst", [128, 512], F32); py = [ps(f"py{i}", [128, 512], F32) for i in range(2)]
    pT = ps("pT", [128, 8, 128], F32); plog = ps("plog", [128, 512], F32)
    XK = Trk()
    for (t0, n) in TCH:
        for c in range(6):
            o.dma(O_[:, c, 0:n], OTi[c * 128:(c + 1) * 128, t0:t0 + n], [], [O_.k])
        for c in range(2):
            o.dma(yf[:, 0:n], LYi[0, c * 128:(c + 1) * 128, t0:t0 + n], [], [yf.k])
            o.dma(yb[:, 0:n], LYi[1, c * 128:(c + 1) * 128, t0:t0 + n], [], [yb.k])
            o.dma(lgt[:, 0:n], LGi[c * 128:(c + 1) * 128, t0:t0 + n], [], [lgt.k])
            o.tt(yf[:, 0:n], yf[:, 0:n], yb[:, 0:n], ALU.add, [yf.k, yb.k], [yf.k])
            o.act(lgt[:, 0:n], lgt[:, 0:n], AF.Gelu, [lgt.k], [lgt.k])
            o.tt(O_[:, 6 + c, 0:n], yf[:, 0:n], lgt[:, 0:n], ALU.mult, [yf.k, lgt.k], [O_.k])
        for gi in range(4):
            for c in range(2):
                o.act(sq[c][:, 0:n], O_[:, 2 * gi + c, 0:n], AF.Square, [O_.k], [sq[c].k])
            for c in range(2):
                o.mm(pst[:, 0:n], g.ones128[:, :], sq[c][:, 0:n], c == 0, c == 1, [g.ones128.k, sq[c].k], [pst.k])
            o.act(srt[:, 0:n], pst[:, 0:n], AF.Sqrt, [pst.k], [srt.k], scale=1.0 / 256, bias=EPS)
            o.rcp(rs[:, 0:n], srt[:, 0:n], [srt.k], [rs.k])
            for c in range(2):
                cc = 2 * gi + c
                gcol = cpk[:, CP["grp_g"][0] + cc:CP["grp_g"][0] + cc + 1]
                o.stt(yn[:, cc, 0:n], O_[:, cc, 0:n], gcol, rs[:, 0:n], ALU.mult, ALU.mult, [O_.k, rs.k, cpk.k], [yn.k])
        for sub in range(n // 128):
            t = t0 // 128 + sub; w = 0 if t < NTL else 1; i = t % 2
            for hf in range(2):
                for c in range(8):
                    o.mm(py[hf][:, 0:512], yn[:, c, sub * 128:(sub + 1) * 128], wout[:, c, hf * 512:(hf + 1) * 512], c == 0, c == 7,
                         [yn.k, wout.k], [py[hf].k])
            o.dma(xt[i][:, :], iv('x', [[D, 128], [1, D]], t * 128 * D) if t < NTL else iv('ctx', [[D, 128], [1, D]], (t - NTL) * 128 * D), [XK], [xt[i].k])
            for hf in range(2):
                o.tt(tA[:, hf * 512:(hf + 1) * 512], py[hf][:, 0:512], GT1[w][:, hf * 512:(hf + 1) * 512], ALU.mult, [py[hf].k, GT1[w].k], [tA.k])
            o.tt(x1[i][:, :], xt[i][:, :], tA[:, :], ALU.add, [xt[i].k, tA.k], [x1[i].k])
            o.dma(X1[t * 128:(t + 1) * 128, :], x1[i][:, :], [x1[i].k], [DK(X1)])
            o.act(tB[:, :], x1[i][:, :], AF.Square, [x1[i].k], [tB.k, ssq.k], accum_out=ssq[:, i:i + 1])
            o.act(srt1[:, i:i + 1], ssq[:, i:i + 1], AF.Sqrt, [ssq.k], [srt1.k], scale=1.0 / D, bias=EPS)
            o.rcp(rs1[:, i:i + 1], srt1[:, i:i + 1], [srt1.k], [rs1.k])
            o.stt(tB[:, :], x1[i][:, :], rs1[:, i:i + 1], G2[w][:, :], ALU.mult, ALU.mult, [x1[i].k, rs1.k, G2[w].k], [tB.k])
            o.tt(h2[i][:, :], tB[:, :], SH2[w][:, :], ALU.add, [tB.k, SH2[w].k], [h2[i].k])
            o.cp(h2b[i][:, :], h2[i][:, :], [h2[i].k], [h2b[i].k], eng="pool")
            o.dma(H2[t * 128:(t + 1) * 128, :], h2b[i][:, :], [h2b[i].k], [DK(H2)])
            for k in range(8):
                o.tr(pT[:, k, :], h2[i][:, k * 128:(k + 1) * 128], g.ident_f[:, :], [h2[i].k, g.ident_f.k], [pT.k])
            o.cp(h2T[:, :, :], pT[:, :, :], [pT.k], [h2T.k])
            for k in range(8):
                o.mm(plog[:, 0:32], h2T[:, k, :], rw[:, k, :], k == 0, k == 7, [h2T.k, rw.k], [plog.k])
            o.tt(lg32[:, :], plog[:, 0:32], rb[:, :], ALU.add, [plog.k, rb.k], [lg32.k])
            kb.op("dve", lambda e: e.max(out=v8[:, :], in_=lg32[:, :]), [lg32.k], [v8.k])
            kb.op("dve", lambda e: e.max_index(out=i8[:, :], in_max=v8[:, :], in_values=lg32[:, :]), [v8.k, lg32.k], [i8.k])
            o.ts(nv0[:, :], v8[:, 0:1], -1.0, None, ALU.mult, ALU.bypass, [v8.k], [nv0.k])
            o.act(e4[:, :], v8[:, 0:4], AF.Exp, [v8.k, nv0.k], [e4.k, se.k], bias=nv0[:, 0:1], accum_out=se[:, 0:1])
            o.rcp(rse[:, :], se[:, :], [se.k], [rse.k])
            R = rtt[i]
            o.ts(R[:, 4:8], e4[:, :], rse[:, 0:1], None, ALU.mult, ALU.bypass, [e4.k, rse.k], [R.k])
            o.cp(R[:, 0:4], i8[:, 0:4], [i8.k], [R.k])
            o.dma(RT[t * 128:(t + 1) * 128, :], R[:, :], [R.k], [DK(RT)])


NTOK = S + CT
def emit_p4(g):
    nc, kb, o, sb, ps, DK, iv = g.nc, g.kb, g.o, g.sb, g.ps, g.DK, g.iv
    d = g.dram
    I = lambda n, s, dt: d(n, s, dt, "ExternalInput")
    H2T = I("h2t", [D, NTOK], BF16); RTi = I("rt", [NTOK, 8], F32)
    WGU = I("wgu", [ELOC, D, 2048], F32); WD = I("wd", [ELOC, D, D], F32)
    FP = g.out("FP", [NTOK, D], BF16)
    wgu = sb("wgu_b", [128, 8, 2048], BF16); wd = sb("wd_b", [128, 8, 1024], BF16)
    bgu = sb("bgu", [128, 16], F32); bdn = sb("bdn", [128, 1024], F32); eid = sb("eid", [128, 4], F32)
    hT = [sb(f"hT{i}", [128, 8, 512], BF16) for i in range(2)]
    actT = sb("actT", [128, 8, 512], BF16)
    xg = [sb(f"xg{i}", [128, 512], F32) for i in range(2)]; sg = [sb(f"sg{i}", [128, 512], F32) for i in range(2)]
    xl = [sb(f"xl{i}", [128, 512], F32) for i in range(2)]
    rt = sb("rt_sb", [128, 4, 8], F32); eq = sb("eq", [128, 4, 4], F32); gate = sb("gate", [128, 4, 1], F32)
    ft = [sb(f"ft{i}", [128, 1024], BF16) for i in range(2)]; yt = [sb(f"yt{i}", [128, 1024], F32) for i in range(2)]
    ytb = [sb(f"ytb{i}", [128, 1024], BF16) for i in range(2)]
    pg = [ps(f"pg{i}", [128, 512], F32) for i in range(4)]; pd = [ps(f"pd{i}", [128, 512], F32) for i in range(4)]
    o.dma(eid[:, :], iv("eid", [[0, 128], [1, 4]]), [], [eid.k])
    chunks = [(c * 512, 512) for c in range(NTOK // 512)] + [(NTOK // 512 * 512, NTOK % 512)]
    for e in range(ELOC):
        for k in range(8):
            o.dma(wgu[:, k, :], WGU[e, k * 128:(k + 1) * 128, :], [], [wgu.k], q="pool")
            o.dma(wd[:, k, :], WD[e, k * 128:(k + 1) * 128, :], [], [wd.k], q="pool")
        o.dma(bgu[:, :], iv("bgu", [[16, 128], [1, 16]], e * 2048), [], [bgu.k])
        o.dma(bdn[:, :], iv("bd", [[0, 128], [1, 1024]], e * 1024), [], [bdn.k])
        for ci, (t0, n) in enumerate(chunks):
            H = hT[ci % 2]
            o.dma(H[:, :, 0:n], dap(H2T, t0, [[NTOK, 128], [128 * NTOK, 8], [1, n]]), [], [H.k])
            nsub = n // 128
            o.dma(rt[:, 0:nsub, :], dap(RTi, t0 * 8, [[8, 128], [128 * 8, nsub], [1, 8]]), [], [rt.k])
            o.ts(eq[:, 0:nsub, :], rt[:, 0:nsub, 0:4], eid[:, e:e + 1], None, ALU.is_equal, ALU.bypass, [rt.k, eid.k], [eq.k])
            o.tt(eq[:, 0:nsub, :], eq[:, 0:nsub, :], rt[:, 0:nsub, 4:8], ALU.mult, [eq.k, rt.k], [eq.k])
            o.red(gate[:, 0:nsub, 0], eq[:, 0:nsub, :], ALU.add, [eq.k], [gate.k])
            for c in range(8):
                i = c % 2
                for half, P_ in ((0, pg[2 * i]), (1, pg[2 * i + 1])):
                    cc = c + 8 * half
                    for k in range(8):
                        o.mm(P_[:, 0:n], wgu[:, k, cc * 128:(cc + 1) * 128], H[:, k, 0:n], k == 0, k == 7, [wgu.k, H.k], [P_.k])
                o.ts(xg[i][:, 0:n], pg[2 * i][:, 0:n], bgu[:, c:c + 1], 7.0, ALU.add, ALU.min, [pg[2 * i].k, bgu.k], [xg[i].k])
                o.act(sg[i][:, 0:n], xg[i][:, 0:n], AF.Sigmoid, [xg[i].k], [sg[i].k], scale=1.702)
                o.ts(xl[i][:, 0:n], pg[2 * i + 1][:, 0:n], bgu[:, 8 + c:9 + c], 7.0, ALU.add, ALU.min, [pg[2 * i + 1].k, bgu.k], [xl[i].k])
                o.ts(xl[i][:, 0:n], xl[i][:, 0:n], -7.0, 1.0, ALU.max, ALU.add, [xl[i].k], [xl[i].k])
                o.tt(xg[i][:, 0:n], xg[i][:, 0:n], sg[i][:, 0:n], ALU.mult, [xg[i].k, sg[i].k], [xg[i].k])
                o.tt(actT[:, c, 0:n], xg[i][:, 0:n], xl[i][:, 0:n], ALU.mult, [xg[i].k, xl[i].k], [actT.k])
            for sub in range(nsub):
                i = sub % 2
                r0 = t0 + sub * 128
                if e > 0:
                    o.dma(ft[i][:, :], FP[r0:r0 + 128, :], [DK(FP)], [ft[i].k])
                for hf in range(2):
                    P_ = pd[2 * i + hf]
                    for k in range(8):
                        o.mm(P_[:, 0:512], actT[:, k, sub * 128:(sub + 1) * 128], wd[:, k, hf * 512:(hf + 1) * 512], k == 0, k == 7,
                             [actT.k, wd.k], [P_.k])
                    o.tt(yt[i][:, hf * 512:(hf + 1) * 512], P_[:, 0:512], bdn[:, hf * 512:(hf + 1) * 512], ALU.add, [P_.k, bdn.k], [yt[i].k])
                if e > 0:
                    o.stt(ytb[i][:, :], yt[i][:, :], gate[:, sub, 0:1], ft[i][:, :], ALU.mult, ALU.add, [yt[i].k, gate.k, ft[i].k], [ytb[i].k])
                else:
                    o.ts(ytb[i][:, :], yt[i][:, :], gate[:, sub, 0:1], None, ALU.mult, ALU.bypass, [yt[i].k, gate.k], [ytb[i].k])
                o.dma(FP[r0:r0 + 128, :], ytb[i][:, :], [ytb[i].k], [DK(FP)])


def emit_p5(g):
    nc, kb, o, sb, ps, DK, iv = g.nc, g.kb, g.o, g.sb, g.ps, g.DK, g.iv
    d = g.dram
    FPi = d("fp", [NCORE, TT, D], BF16, "ExternalInput"); X1i = d("x1", [TT, D], F32, "ExternalInput")
    X2 = g.out("X2", [TT, D], F32)
    GT2 = [sb(f"GT2{w}", [128, 1024], F32) for w in range(2)]
    for w in range(2):
        o.dma(GT2[w][:, :], iv("modv", [[0, 128], [1, 1024]], (w * 6 + 5) * 1024), [], [GT2[w].k])
    fin = [sb(f"fin{i}", [128, 1024], BF16) for i in range(4)]; acc = [sb(f"acc{i}", [128, 1024], F32) for i in range(2)]
    xt = [sb(f"xt{i}", [128, 1024], F32) for i in range(2)]
    for t in range(NT):
        w = 0 if t < NTL else 1; i = t % 2; A = acc[i]
        for c in range(NCORE):
            F_ = fin[c % 4]
            o.dma(F_[:, :], FPi[c, t * 128:(t + 1) * 128, :], [], [F_.k])
            if c == 0:
                o.cp(A[:, :], F_[:, :], [F_.k], [A.k])
            else:
                o.tt(A[:, :], A[:, :], F_[:, :], ALU.add, [A.k, F_.k], [A.k])
        o.dma(xt[i][:, :], X1i[t * 128:(t + 1) * 128, :], [], [xt[i].k])
        o.tt(A[:, :], A[:, :], GT2[w][:, :], ALU.mult, [A.k, GT2[w].k], [A.k])
        o.tt(A[:, :], A[:, :], xt[i][:, :], ALU.add, [A.k, xt[i].k], [A.k])
        o.dma(X2[t * 128:(t + 1) * 128, :], A[:, :], [A.k], [DK(X2)])


def host_p3(inp, l, x, xc, mod, p1, p2):
    maps = []
    lys = [np.asarray(p2[gb]["LY"], np.float32) for gb in range(4)]
    for j in range(NCORE):
        m = {"ot": np.asarray(p2[j]["OT"]), "lg": np.asarray(p1[j]["LG"])}
        ly = np.empty((2, 256, TT), np.float32)
        for gb in range(4):
            L_ = lys[gb]
            ly[0, gb * 64:(gb + 1) * 64, :TL] = L_[0][:, CT + j * TL:CT + (j + 1) * TL]
            ly[0, gb * 64:(gb + 1) * 64, TL:] = L_[0][:, :CT]
            ly[1, gb * 64:(gb + 1) * 64, :TL] = L_[1][:, CT:][:, ::-1][:, j * TL:(j + 1) * TL]
            ly[1, gb * 64:(gb + 1) * 64, TL:] = L_[1][:, :CT][:, ::-1]
        m["ly"] = ly
        pk = Packer()
        add_consts(pk)
        pk.add("w_out", inp["w_out"][l]); pk.add("router_w", inp["router_w"][l]); pk.add("router_b", inp["router_b"][l])
        pk.add("norm2_g", inp["norm2_g"][l]); pk.add("cp", make_cp(inp, l))
        pk.add("x", x[j * TL:(j + 1) * TL]); pk.add("ctx", xc)
        pk.add("modv", mod[:, l, :].reshape(2, 6, 1024))
        arr, irows = pk.finish_single()
        m["inp"] = arr
        maps.append({k: np.ascontiguousarray(v) for k, v in m.items()})
    if "p3" not in _CACHE: _CACHE["p3"] = build(emit_p3, pk.items, irows)
    return run(_CACHE["p3"], maps)


def host_p4(inp, l, p3):
    h2 = np.concatenate([np.asarray(r["H2"])[:TL] for r in p3] + [np.asarray(p3[0]["H2"])[TL:]], axis=0)
    h2t = np.ascontiguousarray(h2.T)
    rt = np.ascontiguousarray(np.concatenate([np.asarray(r["RT"])[:TL] for r in p3] + [np.asarray(p3[0]["RT"])[TL:]], axis=0))
    maps = []
    for j in range(NCORE):
        es = slice(j * ELOC, (j + 1) * ELOC)
        pk = Packer()
        pk.add("eid", np.arange(j * ELOC, (j + 1) * ELOC, dtype=np.float32))
        pk.add("bgu", np.stack([inp["exp_b_gu"][l][e].reshape(16, 128).T for e in range(j * ELOC, (j + 1) * ELOC)]))
        pk.add("bd", inp["exp_b_down"][l][es])
        arr, irows = pk.finish_single()
        maps.append({"inp": arr, "h2t": h2t, "rt": rt,
                     "wgu": np.ascontiguousarray(inp["exp_w_gu"][l][es], dtype=np.float32),
                     "wd": np.ascontiguousarray(inp["exp_w_down"][l][es], dtype=np.float32)})
    if "p4" not in _CACHE: _CACHE["p4"] = build(emit_p4, pk.items, irows)
    return run(_CACHE["p4"], maps)


def host_p5(mod, l, p3, p4):
    fps = [np.asarray(r["FP"]) for r in p4]
    maps = []
    for j in range(NCORE):
        fp = np.stack([np.concatenate([f[j * TL:(j + 1) * TL], f[S:S + CT]], axis=0) for f in fps])
        pk = Packer()
        pk.add("modv", mod[:, l, :].reshape(2, 6, 1024))
        arr, irows = pk.finish_single()
        maps.append({"inp": arr, "fp": fp, "x1": np.asarray(p3[j]["X1"])})
    if "p5" not in _CACHE: _CACHE["p5"] = build(emit_p5, pk.items, irows)
    return run(_CACHE["p5"], maps)


def kernel(**inputs):
    inp = {k: np.asarray(v) for k, v in inputs.items()}
    mod = host_p0(inp)
    x = np.asarray(inp["x"], np.float32).reshape(S, D)
    xc = np.asarray(inp["ctx"], np.float32).reshape(CT, D)
    for l in range(L):
        p1 = host_p1(inp, l, x, xc, mod)
        p2 = host_p2(inp, l, p1)
        p3 = host_p3(inp, l, x, xc, mod, p1, p2)
        del p2
        p4 = host_p4(inp, l, p3)
        p5 = host_p5(mod, l, p3, p4)
        del p1, p3, p4
        x = np.concatenate([np.asarray(r["X2"])[:TL] for r in p5], axis=0)
        xc = np.ascontiguousarray(np.asarray(p5[0]["X2"])[TL:])
    return np.ascontiguousarray(x, dtype=np.float32).reshape(1, S, D)
```

```python
import contextlib
import numpy as np
import concourse.bass as bass
import concourse.mybir as mybir
from concourse.bass_utils import run_bass_kernel_spmd

F32 = mybir.dt.float32; BF16 = mybir.dt.bfloat16; U32 = mybir.dt.uint32; I32 = mybir.dt.int32
AF = mybir.ActivationFunctionType; ALU = mybir.AluOpType; AX = mybir.AxisListType

NCORE = 8; D = 1024; S = 16384; TL = 2048; CT = 256; TT = TL + CT; NTL = 16; NT = 18; L = 4
GW = 64; EPS = 1e-6
NE = 32; ELOC = 4; CAP = 2560; NTOK_ALL = NCORE * TT
NTILE_ALL = NTOK_ALL // 128
BW = 512


class Trk:
    __slots__ = ("w", "r")
    def __init__(self):
        self.w = None; self.r = []


class KB:
    ENG = ("pe", "act", "dve", "pool", "sp")
    SEMKEYS = ("c_pe", "c_act", "c_dve", "c_pool", "d_sp", "d_pool", "d_cc")
    def __init__(self, nc):
        self.nc = nc
        self.streams = {e: [] for e in self.ENG}
        self.cnt = {e: 0 for e in self.ENG}
        self.dcnt = {"sp": 0, "pool": 0, "cc": 0}
        self.seen = {e: {} for e in self.ENG}
    def _needs(self, reads, writes):
        ev = []
        for t in reads:
            if t.w is not None: ev.append(t.w)
        for t in writes:
            if t.w is not None: ev.append(t.w)
            ev.extend(t.r)
        return ev
    def _waits(self, e, events):
        need = {}
        for (sk, val, src) in events:
            if src == "pe" and e == "pe" and sk == "c_pe": continue
            if self.seen[e].get(sk, 0) >= val: continue
            if need.get(sk, 0) < val: need[sk] = val
        for sk, val in need.items(): self.seen[e][sk] = val
        return list(need.items())
    def _upd(self, evt, reads, writes):
        for t in writes:
            t.w = evt; t.r = []
        for t in reads:
            if t not in writes: t.r.append(evt)
    def op(self, e, fn, reads=(), writes=()):
        waits = self._waits(e, self._needs(reads, writes))
        self.cnt[e] += 1
        sk = "c_" + e
        evt = (sk, self.cnt[e], e)
        self.streams[e].append((waits, fn, (sk, 1)))
        self._upd(evt, reads, writes)
    def dma(self, q, fn, reads=(), writes=(), cc=False):
        waits = self._waits(q, self._needs(reads, writes))
        key = "cc" if cc else q
        inc = 1 if cc else 16
        self.dcnt[key] += inc
        sk = "d_" + key
        evt = (sk, self.dcnt[key], q)
        self.streams[q].append((waits, fn, (sk, inc)))
        self.streams[q].append(([(sk, self.dcnt[key])], None, None))
        self.seen[q][sk] = self.dcnt[key]
        self._upd(evt, reads, writes)
    def wait_all(self, e, trks):
        ev = []
        for t in trks:
            if t.w is not None: ev.append(t.w)
            ev.extend(t.r)
        waits = self._waits(e, ev)
        if waits: self.streams[e].append((waits, None, None))
    def emit(self, sems):
        nc = self.nc
        with nc.Block() as block:
            def run(eng, lst):
                for waits, fn, inc in lst:
                    for sk, val in waits: eng.wait_ge(sems[sk], val)
                    if fn is not None:
                        ins = fn(eng)
                        ins.then_inc(sems[inc[0]], inc[1])
            if self.streams["pe"]:
                @block.tensor
                def _(eng): run(eng, self.streams["pe"])
            if self.streams["act"]:
                @block.scalar
                def _(eng): run(eng, self.streams["act"])
            if self.streams["dve"]:
                @block.vector
                def _(eng): run(eng, self.streams["dve"])
            if self.streams["pool"]:
                @block.gpsimd
                def _(eng): run(eng, self.streams["pool"])
            if self.streams["sp"]:
                @block.sync
                def _(eng): run(eng, self.streams["sp"])


class Til:
    def __init__(self, t):
        self.t = t; self.k = Trk()
    def __getitem__(self, idx):
        return self.t[idx]


def _rope_tables(n_tok_start, n_tok, rot_dim):
    t = np.arange(n_tok_start, n_tok_start + n_tok)
    row = (t // GW).astype(np.float64); col = (t % GW).astype(np.float64)
    ax = rot_dim // 2
    inv = 10000.0 ** (-np.arange(0, ax, 2, dtype=np.float64) / ax)
    h = rot_dim // 4
    cos = np.zeros((rot_dim, n_tok)); sin = np.zeros((rot_dim, n_tok))
    for d in range(rot_dim):
        axis = d // (2 * h); half = (d // h) % 2; j = d % h
        ang = (row if axis == 0 else col) * inv[j]
        cos[d] = np.cos(ang.astype(np.float32).astype(np.float64))
        sin[d] = np.sin(ang.astype(np.float32).astype(np.float64)) * (-1.0 if half == 0 else 1.0)
    return cos.astype(np.float32), sin.astype(np.float32)


def _rope_perm(rot_dim, n, base=0, reps=1, stride=None):
    P = np.zeros((n, n), np.float32)
    h = rot_dim // 4
    stride = stride or rot_dim
    for r in range(reps):
        for d in range(rot_dim):
            half = (d // h) % 2
            pd = d + h if half == 0 else d - h
            P[base + r * stride + pd, base + r * stride + d] = 1.0
    return P


class Packer:
    def __init__(self):
        self.items = {}; self.parts = []; self.off = 0
    def add(self, name, arr):
        a = np.ascontiguousarray(np.asarray(arr, dtype=np.float32))
        self.items[name] = (self.off, tuple(a.shape))
        self.parts.append(a.reshape(-1)); self.off += a.size
        pad = (-self.off) % 64
        if pad:
            self.parts.append(np.zeros(pad, np.float32)); self.off += pad
    def finish_single(self):
        rows = -(-self.off // BW)
        flat = np.concatenate(self.parts + [np.zeros(rows * BW - self.off, np.float32)])
        return flat.reshape(rows, BW), rows
    def finish(self):
        per = -(-self.off // (NCORE * BW))
        tot = per * NCORE * BW
        flat = np.concatenate(self.parts + [np.zeros(tot - self.off, np.float32)])
        return flat.reshape(NCORE, per, BW), per


CP = {}
def _cp_layout():
    names = [("na_qn2", 1), ("gqa_qn2", 1), ("gqa_kn2", 1), ("mla_qa_g", 2), ("mla_kva_g", 1), ("mla_qn", 1),
             ("mla_knn2", 1), ("mla_knr", 1), ("conv_w", 8), ("conv_b", 2), ("lru_ba", 4), ("lru_bi", 4),
             ("lru_lam", 4), ("grp_g", 8), ("sc96", 1)]
    o = 0
    for n, w in names:
        CP[n] = (o, w); o += w
    return o
NCOL = _cp_layout()


def add_consts(pk):
    ones64 = np.kron(np.eye(2, dtype=np.float32), np.ones((64, 64), np.float32))
    pk.add("ones64blk", ones64)
    pk.add("ones128", np.ones((128, 128), np.float32))
    b96 = np.zeros((128, 128), np.float32); b96[:64, :64] = 1; b96[64:96, 64:96] = 1
    pk.add("blk96", b96)
    pk.add("ident", np.eye(128, dtype=np.float32))
    pk.add("antiid", np.eye(128, dtype=np.float32)[::-1])
    pk.add("prot64", _rope_perm(64, 128, 0, 2))
    p96 = np.zeros((128, 128), np.float32); p96[64:96, 64:96] = _rope_perm(32, 32)
    pk.add("prot96", p96)
    pk.add("prot32", np.pad(_rope_perm(32, 32), ((0, 96), (0, 96))))


def make_cp(inp, l):
    cp = np.zeros((128, NCOL), np.float32)
    def put(name, mat):
        o, w = CP[name]
        mat = np.asarray(mat, np.float32)
        if mat.ndim == 1: mat = mat.reshape(-1, 1)
        cp[:mat.shape[0], o:o + w] = mat
    put("na_qn2", np.tile(inp["na_qn"][l], 2)); put("gqa_qn2", np.tile(inp["gqa_qn"][l], 2))
    put("gqa_kn2", np.tile(inp["gqa_kn"][l], 2))
    put("mla_qa_g", inp["mla_qa_g"][l].reshape(2, 128).T); put("mla_kva_g", inp["mla_kva_g"][l])
    put("mla_qn", inp["mla_qn"][l]); put("mla_knn2", np.tile(inp["mla_kn"][l][:64], 2))
    put("mla_knr", inp["mla_kn"][l][64:])
    put("grp_g", inp["grp_g"][l].reshape(8, 128).T)
    s96 = np.zeros(128, np.float32); s96[:64] = 1.0 / 64; s96[64:96] = 1.0 / 32; s96[96:] = 1.0
    put("sc96", s96)
    return cp


class Ctx:
    pass


def build(emit_fn, iitems, irows, **kw):
    nc = bass.Bass("TRN2", target_bir_lowering=False)
    kb = KB(nc)
    g = Ctx(); g.nc = nc; g.kb = kb; g.iitems = iitems; g.irows = irows
    st = contextlib.ExitStack()
    g.st = st
    with st:
        sems = {k: st.enter_context(nc.semaphore(k)) for k in KB.SEMKEYS}
        _setup(g)
        emit_fn(g, **kw)
        kb.emit(sems)
    return nc


def _setup(g):
    nc, st = g.nc, g.st
    g.o = _ops(g)
    def sb(name, shape, dt, stack=None):
        return Til((stack or st).enter_context(nc.sbuf_tensor(name, list(shape), dt)))
    def ps(name, shape, dt, stack=None):
        return Til((stack or st).enter_context(nc.psum_tensor(name, list(shape), dt)))
    def dram(name, shape, dt, kind=None):
        return nc.dram_tensor(name, list(shape), dt, kind=kind) if kind else nc.dram_tensor(name, list(shape), dt)
    g.sb, g.ps, g.dram = sb, ps, dram
    g.dk = {}
    g.DK = lambda t: g.dk.setdefault(t.name, Trk())
    g.inp = dram("inp", [g.irows, BW], F32, "ExternalInput")
    def iv(name, dims, extra=0):
        off, _ = g.iitems[name]
        return dap(g.inp, off + extra, dims)
    g.iv = iv
    g.bv = iv
    g.BK = Trk()
    g.outs = {}
    def out(name, shape, dt):
        t = dram(name, shape, dt, "ExternalOutput"); g.outs[name] = t
        return t
    g.out = out


def load_consts(g, names):
    o = g.o
    for nm in names:
        tl = g.sb("c_" + nm, [128, 128], F32)
        o.dma(tl[:, :], g.iv(nm, [[128, 128], [1, 128]]), [], [tl.k])
        setattr(g, {"ident": "ident_f", "ones64blk": "ones64"}.get(nm, nm), tl)
    if "ident" in names:
        g.ident_b = g.sb("c_ident_b", [128, 128], BF16)
        o.cp(g.ident_b[:, :], g.ident_f[:, :], [g.ident_f.k], [g.ident_b.k])


def emit_p0(g, nlayers=L):
    o, sb, ps, iv = g.o, g.sb, g.ps, g.iv
    modo = g.out("mod", [2, nlayers * 768], F32)
    cT = sb("cT", [128, 16], F32); scT = sb("scT", [128, 16], F32)
    aw = sb("aw", [128, 8, 768], F32); ab = sb("ab", [2, nlayers * 768], F32)
    msb = sb("msb", [2, nlayers * 768], F32)
    pm = [ps(f"pm{i}", [128, 512], F32) for i in range(2)]
    o.dma(cT[:, :], iv("cT2", [[16, 128], [1, 16]]), [], [cT.k])
    o.act(scT[:, :], cT[:, :], AF.Silu, [cT.k], [scT.k])
    o.dma(ab[:, :], iv("adab", [[0, 2], [1, nlayers * 768]]), [], [ab.k])
    for l in range(nlayers):
        o.dma(aw[:, :, :], iv("adaw", [[768, 128], [128 * 768, 8], [1, 768]], l * D * 768), [], [aw.k])
        for ci, (c0, cn) in enumerate(((0, 512), (512, 256))):
            for k in range(8):
                o.mm(pm[ci][0:2, 0:cn], scT[:, k:16:8], aw[:, k, c0:c0 + cn], k == 0, k == 7, [scT.k, aw.k], [pm[ci].k])
            o.tt(msb[:, l * 768 + c0:l * 768 + c0 + cn], pm[ci][0:2, 0:cn], ab[:, l * 768 + c0:l * 768 + c0 + cn], ALU.add,
                 [pm[ci].k, ab.k], [msb.k])
    o.dma(modo.ap(), msb[:, :], [msb.k], [g.DK(modo)])


def emit_p1(g):
    load_consts(g, ["ident", "ones64blk", "ones128", "blk96", "prot64", "prot96", "prot32"])
    def load_modvec(dst, l, w, v, q="sp"):
        g.o.dma(dst[:, :], g.iv("modv", [[0, 128], [1, 1024]], (w * 6 + v) * 1024), [], [dst.k], q=q)
    g.load_modvec = load_modvec
    _stage_a(g, "")


def _ops(g):
    kb = g.kb
    def mm(out, lhsT, rhs, start, stop, reads, writes):
        kb.op("pe", lambda e: e.matmul(out, lhsT=lhsT, rhs=rhs, start=start, stop=stop), reads, writes)
    def tr(out, in_, ident, reads, writes):
        kb.op("pe", lambda e: e.transpose(out, in_, ident), reads, writes)
    def act(out, in_, func, reads, writes, eng="act", **kw):
        kb.op(eng, lambda e: e.activation(out=out, in_=in_, func=func, **kw), reads, writes)
    def ts(out, in0, s1, s2, op0, op1, reads, writes, eng="dve"):
        kb.op(eng, lambda e: e.tensor_scalar(out=out, in0=in0, scalar1=s1, scalar2=s2, op0=op0, op1=op1), reads, writes)
    def stt(out, in0, scalar, in1, op0, op1, reads, writes):
        kb.op("dve", lambda e: e.scalar_tensor_tensor(out=out, in0=in0, scalar=scalar, in1=in1, op0=op0, op1=op1), reads, writes)
    def tt(out, in0, in1, op, reads, writes, eng="dve"):
        kb.op(eng, lambda e: e.tensor_tensor(out=out, in0=in0, in1=in1, op=op), reads, writes)
    def cp(out, in_, reads, writes, eng="dve"):
        kb.op(eng, lambda e: e.tensor_copy(out=out, in_=in_), reads, writes)
    def rcp(out, in_, reads, writes):
        kb.op("dve", lambda e: e.reciprocal(out=out, in_=in_), reads, writes)
    def red(out, in_, op, reads, writes):
        kb.op("dve", lambda e: e.tensor_reduce(out=out, in_=in_, axis=AX.X, op=op), reads, writes)
    def ms(ap, val, writes, eng="pool"):
        kb.op(eng, lambda e: e.memset(ap, val), (), writes)
    def dma(out, in_, reads, writes, q="sp", **kw):
        kb.dma(q, lambda e: e.dma_start(out=out, in_=in_, **kw), reads, writes)
    def ag(out_t, in_t, reads, writes, kind="AllGather", op=ALU.bypass):
        kb.dma("pool", lambda e: e.collective_compute(kind, op, replica_groups=[list(range(NCORE))],
                                                       ins=[in_t.ap().opt()], outs=[out_t.ap().opt()]),
               reads, writes, cc=True)
        kb.op("pool", lambda e: e.memset(g.dummy[:, :], 0.0), (), list(writes) + [g.dummy.k])
    o = Ctx()
    o.mm, o.tr, o.act, o.ts, o.stt, o.tt, o.cp, o.rcp, o.red, o.ms, o.dma, o.ag = mm, tr, act, ts, stt, tt, cp, rcp, red, ms, dma, ag
    return o


def dap(t, off, dims):
    return bass.AP(tensor=t, offset=off, ap=[list(d) for d in dims])


DBG_SHAPES = {"GQ": (256, TT), "GK_src": (128, TL), "GKC": (128, CT), "NQ": (256, TT), "MQ": (384, TT), "MKN_src": (256, TL),
              "MKNC": (256, CT), "MKR_src": (32, TL), "MKRC": (32, CT), "LX": (256, TT), "LG": (256, TT), "NAK_src": (TL, 256),
              "NAV_src": (TL, 260), "GV_src": (TL, 130), "MV_src": (TL, 260), "LXH_src": (256, 256), "GK_all": (NCORE * 128, TL),
              "NAK_all": (S, 256), "MV_all": (S, 260), "mod_all": (16, 768), "x_cur": (TT, D), "OT": (D, TT), "F_loc": (TT, D)}
import os
SUB = int(os.environ.get('K_SUB', '0'))
TCH = [(0, 512), (512, 512), (1024, 512), (1536, 512), (2048, 256)]


def _layer_dram(g):
    d = g.out
    g.GQ = d("GQ", [256, TT], BF16); g.GK_src = d("GK_src", [128, TL], BF16); g.GKC = d("GKC", [128, CT], BF16)
    g.NQ = d("NQ", [256, TT], BF16)
    g.MQ = d("MQ", [384, TT], BF16)
    g.MKN_src = d("MKN_src", [256, TL], BF16); g.MKNC = d("MKNC", [256, CT], BF16)
    g.MKR_src = d("MKR_src", [32, TL], BF16); g.MKRC = d("MKRC", [32, CT], BF16)
    g.LX = d("LX", [256, TT], F32); g.LG = d("LG", [256, TT], F32)
    g.NAK_src = d("NAK_src", [TL, 256], BF16); g.NAKC = d("NAKC", [CT, 256], BF16)
    g.NAV_src = d("NAV_src", [TL, 260], BF16); g.NAVC = d("NAVC", [CT, 260], BF16)
    g.GV_src = d("GV_src", [TL, 130], BF16); g.GVC = d("GVC", [CT, 130], BF16)
    g.MV_src = d("MV_src", [TL, 260], BF16); g.MVC = d("MVC", [CT, 260], BF16)


def _stage_a(g, l):
    nc, kb, o, sb, ps, DK, bv = g.nc, g.kb, g.o, g.sb, g.ps, g.DK, g.bv
    _layer_dram(g)
    with contextlib.ExitStack() as ss:
        G = [sb(f"aG{w}", [128, 1024], F32, ss) for w in range(2)]
        SH = [sb(f"aSH{w}", [128, 1024], F32, ss) for w in range(2)]
        tmpA = sb("a_tmpA", [128, 1024], F32, ss); tmpB = sb("a_tmpB", [128, 1024], F32, ss)
        win = sb("a_win", [128, 8, 2208], BF16, ss)
        wuq = sb("a_wuq", [128, 2, 384], BF16, ss); wkn = sb("a_wkn", [128, 256], BF16, ss); wv = sb("a_wv", [128, 256], BF16, ss)
        cpk = sb("a_cpk", [128, NCOL], F32, ss); kn4 = sb("a_kn4", [128, 256], F32, ss)
        hT = sb("a_hT", [128, 8, TT], BF16, ss); hk = [Trk() for _ in range(NT)]
        xt = [sb(f"a_xt{i}", [128, 1024], F32, ss) for i in range(2)]
        hb = [sb(f"a_hb{i}", [128, 1024], BF16, ss) for i in range(2)]
        ssq = sb("a_ssq", [128, 2], F32, ss); srt1 = sb("a_srt1", [128, 2], F32, ss); rs1 = sb("a_rs1", [128, 2], F32, ss)
        rq = sb("a_rq", [128, 2, 512], F32, ss); rm = sb("a_rm", [128, 2, 512], F32, ss); rk = sb("a_rk", [32, 2, 512], F32, ss)
        sq = [sb(f"a_sq{i}", [128, 512], F32, ss) for i in range(2)]
        srt = sb("a_srt", [128, 512], F32, ss); rs = sb("a_rs", [128, 512], F32, ss)
        qn = sb("a_qn", [128, 512], F32, ss); t1 = sb("a_t1", [128, 512], F32, ss); t2 = sb("a_t2", [128, 512], F32, ss)
        ob = [sb(f"a_ob{i}", [128, 512], BF16, ss) for i in range(3)]
        of = [sb(f"a_of{i}", [128, 512], F32, ss) for i in range(2)]
        cqn = sb("a_cqn", [128, 2, 512], BF16, ss); ckvn = sb("a_ckvn", [128, 512], BF16, ss)
        sqk = sb("a_sqk", [128, 256], F32, ss); red4 = sb("a_red4", [128, 4, 1], F32, ss)
        srt4 = sb("a_srt4", [128, 4, 1], F32, ss); rs4 = sb("a_rs4", [128, 4, 1], F32, ss)
        nak = [sb(f"a_nak{i}", [128, 256], BF16, ss) for i in range(2)]
        vN = [sb(f"a_vN{i}", [128, 4, 65], BF16, ss) for i in range(2)]
        vG = [sb(f"a_vG{i}", [128, 2, 65], BF16, ss) for i in range(2)]
        vM = [sb(f"a_vM{i}", [128, 4, 65], BF16, ss) for i in range(2)]
        pu = [ps(f"a_pu{i}", [128, 512], F32, ss) for i in range(3)]
        pst = ps("a_pst", [128, 512], F32, ss); pr = ps("a_pr", [128, 512], F32, ss)
        pT = ps("a_pT", [128, 8, 128], BF16, ss); pvt = ps("a_pvt", [128, 512], F32, ss)

        BK = g.BK
        if SUB == 10: return
        stg = [sb(f"a_stg{i}", [128, 2208], F32, ss) for i in range(2)]
        for k in range(8):
            sg_ = stg[k % 2]
            o.dma(sg_[:, :], bv(f"w_in{l}", [[2208, 128], [1, 2208]], k * 128 * 2208), [BK], [sg_.k])
            o.cp(win[:, k, :], sg_[:, :], [sg_.k], [win.k], eng="pool")
        for c in range(2):
            sg_ = stg[c % 2]
            o.dma(sg_[:, 0:384], bv(f"wuq{l}", [[384, 128], [1, 384]], c * 128 * 384), [BK], [sg_.k])
            o.cp(wuq[:, c, :], sg_[:, 0:384], [sg_.k], [wuq.k], eng="pool")
        sg_ = stg[0]
        o.dma(sg_[:, 0:512], bv(f"wukv{l}", [[512, 128], [1, 512]]), [BK], [sg_.k])
        o.cp(wkn[:, :].rearrange("p (h d) -> p h d", d=64), sg_[:, 0:512].rearrange("p (h t) -> p h t", t=128)[:, :, 0:64], [sg_.k], [wkn.k], eng="pool")
        o.cp(wv[:, :].rearrange("p (h d) -> p h d", d=64), sg_[:, 0:512].rearrange("p (h t) -> p h t", t=128)[:, :, 64:128], [sg_.k], [wv.k], eng="pool")
        if SUB == 11: return
        o.dma(cpk[:, :], bv(f"cp{l}", [[NCOL, 128], [1, NCOL]]), [BK], [cpk.k])
        o.dma(kn4[:, :], bv(f"na_kn4{l}", [[0, 128], [1, 256]]), [BK], [kn4.k])
        def col(name, i=0, rows=128):
            c = CP[name][0] + i
            return cpk[0:rows, c:c + 1]
        if SUB == 12: return
        o.dma(tmpA[:, :], bv(f"norm1_g{l}", [[0, 128], [1, 1024]]), [BK], [tmpA.k])
        if SUB == 121: return
        for w in range(2):
            g.load_modvec(tmpB, l, w, 1)
            if SUB == 122: return
            o.stt(G[w][:, :], tmpB[:, :], 1.0, tmpA[:, :], ALU.add, ALU.mult, [tmpB.k, tmpA.k], [G[w].k])
            if SUB == 123: return
            g.load_modvec(SH[w], l, w, 0)
        if SUB == 13: return
        for i in range(2):
            o.ms(vN[i][:, :, :], 1.0, [vN[i].k]); o.ms(vG[i][:, :, :], 1.0, [vG[i].k]); o.ms(vM[i][:, :, :], 1.0, [vM[i].k])

        if SUB == 1: return
        XK = Trk()
        for t in range(NT):
            w = 0 if t < NTL else 1; i = t % 2
            o.dma(xt[i][:, :], g.iv('x', [[D, 128], [1, D]], t * 128 * D) if t < NTL else g.iv('ctx', [[D, 128], [1, D]], (t - NTL) * 128 * D), [XK], [xt[i].k])
            o.act(tmpA[:, :], xt[i][:, :], AF.Square, [xt[i].k], [tmpA.k, ssq.k], accum_out=ssq[:, i:i + 1])
            o.act(srt1[:, i:i + 1], ssq[:, i:i + 1], AF.Sqrt, [ssq.k], [srt1.k], scale=1.0 / D, bias=EPS)
            o.rcp(rs1[:, i:i + 1], srt1[:, i:i + 1], [srt1.k], [rs1.k])
            o.stt(tmpB[:, :], xt[i][:, :], rs1[:, i:i + 1], G[w][:, :], ALU.mult, ALU.mult, [xt[i].k, rs1.k, G[w].k], [tmpB.k])
            o.tt(hb[i][:, :], tmpB[:, :], SH[w][:, :], ALU.add, [tmpB.k, SH[w].k], [hb[i].k])
            for k in range(8):
                o.tr(pT[:, k, :], hb[i][:, k * 128:(k + 1) * 128], g.ident_b[:, :], [hb[i].k, g.ident_b.k], [pT.k])
            o.cp(hT[:, :, t * 128:(t + 1) * 128], pT[:, :, :], [pT.k], [hk[t]])

        if SUB == 2: return
        def hks(t0, n):
            return [hk[t] for t in range(t0 // 128, (t0 + n) // 128)]
        def proj(pt, col0, M, t0, n):
            for k in range(8):
                o.mm(pt[0:M, 0:n], win[:, k, col0:col0 + M], hT[:, k, t0:t0 + n], k == 0, k == 7, [win.k] + hks(t0, n), [pt.k])
        def headnorm(pu_, M, n, ones_ap, ones_k, scale, gcol, out_t, sqi=0):
            o.act(sq[sqi][0:M, 0:n], pu_[0:M, 0:n], AF.Square, [pu_.k], [sq[sqi].k])
            o.mm(pst[0:M, 0:n], ones_ap, sq[sqi][0:M, 0:n], True, True, [ones_k, sq[sqi].k], [pst.k])
            o.act(srt[0:M, 0:n], pst[0:M, 0:n], AF.Sqrt, [pst.k], [srt.k], scale=scale, bias=EPS)
            o.rcp(rs[0:M, 0:n], srt[0:M, 0:n], [srt.k], [rs.k])
            o.stt(out_t[0:M, 0:n], pu_[0:M, 0:n], gcol, rs[0:M, 0:n], ALU.mult, ALU.mult, [pu_.k, rs.k, cpk.k], [out_t.k])
        def rope(qn_, M, n, prot, tab, out_t):
            o.mm(pr[0:M, 0:n], prot[0:M, 0:M], qn_[0:M, 0:n], True, True, [prot.k, qn_.k], [pr.k])
            o.tt(t1[0:M, 0:n], qn_[0:M, 0:n], tab[0:M, 0, 0:n], ALU.mult, [qn_.k, tab.k], [t1.k])
            o.tt(t2[0:M, 0:n], pr[0:M, 0:n], tab[0:M, 1, 0:n], ALU.mult, [pr.k, tab.k], [t2.k])
            o.tt(out_t[0:M, 0:n], t1[0:M, 0:n], t2[0:M, 0:n], ALU.add, [t1.k, t2.k], [out_t.k])
        obi = [0]
        def nob():
            obi[0] = (obi[0] + 1) % 3
            return ob[obi[0]]

        for (t0, n) in TCH:
            lat = t0 < TL
            if lat:
                o.dma(rq[:, :, 0:n], g.iv("ropeq", [[TL, 128], [128 * TL, 2], [1, n]], t0), [], [rq.k])
                o.dma(rm[:, :, 0:n], g.iv("ropem", [[TL, 128], [128 * TL, 2], [1, n]], t0), [], [rm.k])
                o.dma(rk[:, :, 0:n], g.iv("ropek", [[TL, 32], [32 * TL, 2], [1, n]], t0), [], [rk.k])
            c0 = t0 if lat else t0 - TL
            for c in range(2):
                proj(pu[c], c * 128, 128, t0, n)
                B = nob()
                headnorm(pu[c], 128, n, g.ones64[:, :], g.ones64.k, 1.0 / 64, col("na_qn2"), B)
                o.dma(g.NQ[c * 128:(c + 1) * 128, t0:t0 + n], B[:, 0:n], [B.k], [DK(g.NQ)])
            for c in range(3):
                proj(pu[c], 768 + c * 128, 128, t0, n)
                B = nob()
                gname = "gqa_qn2" if c < 2 else "gqa_kn2"
                if lat:
                    headnorm(pu[c], 128, n, g.ones64[:, :], g.ones64.k, 1.0 / 64, col(gname), qn)
                    rope(qn, 128, n, g.prot64, rq, B)
                else:
                    headnorm(pu[c], 128, n, g.ones64[:, :], g.ones64.k, 1.0 / 64, col(gname), B)
                if c < 2:
                    o.dma(g.GQ[c * 128:(c + 1) * 128, t0:t0 + n], B[:, 0:n], [B.k], [DK(g.GQ)])
                elif lat:
                    o.dma(g.GK_src[:, c0:c0 + n], B[:, 0:n], [B.k], [DK(g.GK_src)])
                else:
                    o.dma(g.GKC[:, c0:c0 + n], B[:, 0:n], [B.k], [DK(g.GKC)])
            for c in range(2):
                proj(pu[c], 1280 + c * 128, 128, t0, n)
                o.act(sq[c][:, 0:n], pu[c][:, 0:n], AF.Square, [pu[c].k], [sq[c].k])
            for c in range(2):
                o.mm(pst[:, 0:n], g.ones128[:, :], sq[c][:, 0:n], c == 0, c == 1, [g.ones128.k, sq[c].k], [pst.k])
            o.act(srt[:, 0:n], pst[:, 0:n], AF.Sqrt, [pst.k], [srt.k], scale=1.0 / 256, bias=EPS)
            o.rcp(rs[:, 0:n], srt[:, 0:n], [srt.k], [rs.k])
            for c in range(2):
                o.stt(cqn[:, c, 0:n], pu[c][:, 0:n], col("mla_qa_g", c), rs[:, 0:n], ALU.mult, ALU.mult, [pu[c].k, rs.k, cpk.k], [cqn.k])
            for h in range(4):
                for c in range(2):
                    o.mm(pu[2][0:96, 0:n], wuq[:, c, 96 * h:96 * h + 96], cqn[:, c, 0:n], c == 0, c == 1, [wuq.k, cqn.k], [pu[2].k])
                B = nob()
                if lat:
                    headnorm(pu[2], 96, n, g.blk96[0:96, 0:96], g.blk96.k, col("sc96", 0, 96), col("mla_qn", 0, 96), qn)
                    rope(qn, 96, n, g.prot96, rm, B)
                else:
                    headnorm(pu[2], 96, n, g.blk96[0:96, 0:96], g.blk96.k, col("sc96", 0, 96), col("mla_qn", 0, 96), B)
                o.dma(g.MQ[h * 96:(h + 1) * 96, t0:t0 + n], B[0:96, 0:n], [B.k], [DK(g.MQ)])
            proj(pu[0], 1536, 128, t0, n)
            o.act(sq[0][:, 0:n], pu[0][:, 0:n], AF.Square, [pu[0].k], [sq[0].k])
            o.mm(pst[:, 0:n], g.ones128[:, :], sq[0][:, 0:n], True, True, [g.ones128.k, sq[0].k], [pst.k])
            o.act(srt[:, 0:n], pst[:, 0:n], AF.Sqrt, [pst.k], [srt.k], scale=1.0 / 128, bias=EPS)
            o.rcp(rs[:, 0:n], srt[:, 0:n], [srt.k], [rs.k])
            o.stt(ckvn[:, 0:n], pu[0][:, 0:n], col("mla_kva_g"), rs[:, 0:n], ALU.mult, ALU.mult, [pu[0].k, rs.k, cpk.k], [ckvn.k])
            for p in range(2):
                o.mm(pu[1][:, 0:n], wkn[:, p * 128:(p + 1) * 128], ckvn[:, 0:n], True, True, [wkn.k, ckvn.k], [pu[1].k])
                B = nob()
                headnorm(pu[1], 128, n, g.ones64[:, :], g.ones64.k, 1.0 / 64, col("mla_knn2"), B)
                if lat:
                    o.dma(g.MKN_src[p * 128:(p + 1) * 128, c0:c0 + n], B[:, 0:n], [B.k], [DK(g.MKN_src)])
                else:
                    o.dma(g.MKNC[p * 128:(p + 1) * 128, c0:c0 + n], B[:, 0:n], [B.k], [DK(g.MKNC)])
            for j in range(n // 128):
                V = vM[j % 2]
                o.mm(pvt[:, 0:256], ckvn[:, j * 128:(j + 1) * 128], wv[:, :], True, True, [ckvn.k, wv.k], [pvt.k])
                o.cp(V[:, :, 0:64], pvt[:, 0:256].rearrange("p (h d) -> p h d", d=64), [pvt.k], [V.k])
                r0 = c0 + j * 128
                dst = g.MV_src if lat else g.MVC
                o.dma(dst[r0:r0 + 128, :], V[:, :, :].rearrange("p h d -> p (h d)"), [V.k], [DK(dst)])
            proj(pu[2], 1664, 32, t0, n)
            B = nob()
            if lat:
                headnorm(pu[2], 32, n, g.ones128[0:32, 0:32], g.ones128.k, 1.0 / 32, col("mla_knr", 0, 32), qn)
                rope(qn, 32, n, g.prot32, rk, B)
                o.dma(g.MKR_src[:, c0:c0 + n], B[0:32, 0:n], [B.k], [DK(g.MKR_src)])
            else:
                headnorm(pu[2], 32, n, g.ones128[0:32, 0:32], g.ones128.k, 1.0 / 32, col("mla_knr", 0, 32), B)
                o.dma(g.MKRC[:, c0:c0 + n], B[0:32, 0:n], [B.k], [DK(g.MKRC)])
            for c in range(4):
                proj(pu[c % 3], 1696 + c * 128, 128, t0, n)
                Fo = of[c % 2]
                o.cp(Fo[:, 0:n], pu[c % 3][:, 0:n], [pu[c % 3].k], [Fo.k], eng="act" if False else "dve")
                dst = g.LX if c < 2 else g.LG
                cc_ = c % 2
                o.dma(dst[cc_ * 128:(cc_ + 1) * 128, t0:t0 + n], Fo[:, 0:n], [Fo.k], [DK(dst)])

        if SUB == 3: return
        for t in range(NT):
            lat = t < NTL; i = t % 2
            r0 = t * 128 if lat else (t - NTL) * 128
            for k in range(8):
                o.mm(pvt[:, 0:512], hT[:, k, t * 128:(t + 1) * 128], win[:, k, 256:768], k == 0, k == 7, [hk[t], win.k], [pvt.k])
            o.act(sqk[:, :], pvt[:, 0:256], AF.Square, [pvt.k], [sqk.k])
            o.red(red4[:, :, 0], sqk[:, :].rearrange("p (h d) -> p h d", d=64), ALU.add, [sqk.k], [red4.k])
            o.act(srt4[:, :, :], red4[:, :, :], AF.Sqrt, [red4.k], [srt4.k], scale=1.0 / 64, bias=EPS)
            o.rcp(rs4[:, :, :], srt4[:, :, :], [srt4.k], [rs4.k])
            o.tt(sqk[:, :].rearrange("p (h d) -> p h d", d=64), pvt[:, 0:256].rearrange("p (h d) -> p h d", d=64),
                 rs4[:, :, 0:1].to_broadcast([128, 4, 64]), ALU.mult, [pvt.k, rs4.k], [sqk.k])
            o.tt(nak[i][:, :], sqk[:, :], kn4[:, :], ALU.mult, [sqk.k, kn4.k], [nak[i].k])
            dst = g.NAK_src if lat else g.NAKC
            o.dma(dst[r0:r0 + 128, :], nak[i][:, :], [nak[i].k], [DK(dst)])
            o.cp(vN[i][:, :, 0:64], pvt[:, 256:512].rearrange("p (h d) -> p h d", d=64), [pvt.k], [vN[i].k])
            dst = g.NAV_src if lat else g.NAVC
            o.dma(dst[r0:r0 + 128, :], vN[i][:, :, :].rearrange("p h d -> p (h d)"), [vN[i].k], [DK(dst)])
            for k in range(8):
                o.mm(pu[0][:, 0:128], hT[:, k, t * 128:(t + 1) * 128], win[:, k, 1152:1280], k == 0, k == 7, [hk[t], win.k], [pu[0].k])
            o.cp(vG[i][:, :, 0:64], pu[0][:, 0:128].rearrange("p (h d) -> p h d", d=64), [pu[0].k], [vG[i].k])
            dst = g.GV_src if lat else g.GVC
            o.dma(dst[r0:r0 + 128, :], vG[i][:, :, :].rearrange("p h d -> p (h d)"), [vG[i].k], [DK(dst)])


_CACHE = {}
def run(nc, maps):
    res = run_bass_kernel_spmd(nc, maps, core_ids=list(range(NCORE)))
    return res.results


def host_p0(inp, nlayers=L):
    c = inp["c"].reshape(D); cc = inp["c_ctx"].reshape(D)
    maps = []
    for j in range(NCORE):
        pk = Packer()
        pk.add("cT2", np.concatenate([c.reshape(8, 128).T, cc.reshape(8, 128).T], axis=1))
        pk.add("adaw", inp["ada_w"][:nlayers, :, j * 768:(j + 1) * 768])
        pk.add("adab", inp["ada_b"][:nlayers, j * 768:(j + 1) * 768])
        arr, irows = pk.finish_single()
        maps.append({"inp": arr})
    nc = build(emit_p0, pk.items, irows, nlayers=nlayers)
    res = run(nc, maps)
    mod = np.concatenate([np.asarray(r["mod"]).reshape(2, nlayers, 768) for r in res], axis=2)
    return mod


def rope_items(pk, j):
    c64, s64 = _rope_tables(j * TL, TL, 64)
    pk.add("ropeq", np.stack([np.concatenate([c64, c64]), np.concatenate([s64, s64])]))
    c32, s32 = _rope_tables(j * TL, TL, 32)
    cm = np.ones((128, TL), np.float32); sm = np.zeros((128, TL), np.float32)
    cm[64:96] = c32; sm[64:96] = s32
    pk.add("ropem", np.stack([cm, sm]))
    pk.add("ropek", np.stack([c32, s32]))


def host_p1(inp, l, x, xc, mod):
    maps = []
    for j in range(NCORE):
        pk = Packer()
        add_consts(pk)
        pk.add("w_in", inp["w_in"][l]); pk.add("wuq", inp["mla_wuq"][l]); pk.add("wukv", inp["mla_wukv"][l])
        pk.add("norm1_g", inp["norm1_g"][l]); pk.add("na_kn4", np.tile(inp["na_kn"][l], 4)); pk.add("cp", make_cp(inp, l))
        pk.add("x", x[j * TL:(j + 1) * TL]); pk.add("ctx", xc)
        pk.add("modv", mod[:, l, :].reshape(2, 6, 1024))
        rope_items(pk, j)
        arr, irows = pk.finish_single()
        maps.append({"inp": arr})
    if "p1" not in _CACHE: _CACHE["p1"] = build(emit_p1, pk.items, irows)
    return run(_CACHE["p1"], maps)


def na_var(lr):
    return 0 if 4 <= lr <= 28 else (1 + lr if lr < 4 else 5 + (lr - 29))


def emit_p2(g):
    nc, kb, o, sb, ps, DK, iv = g.nc, g.kb, g.o, g.sb, g.ps, g.DK, g.iv
    d = g.dram
    I = lambda n, s, dt: d(n, s, dt, "ExternalInput")
    gq = I("gq", [256, TT], BF16); gk = I("gk", [128, S + CT], BF16); gv = I("gv", [S + CT, 130], BF16)
    mq = I("mq", [384, TT], BF16); mk = I("mk", [384, S + CT], BF16); mv = I("mv", [S + CT, 260], BF16)
    nq = I("nq", [256, TT], BF16); nkt = I("nkt", [32, 256, 512], BF16); nv = I("nv", [32, 512, 260], BF16)
    nkc = I("nkc", [256, CT], BF16); nvc = I("nvc", [CT, 260], BF16)
    eb = I("ebias", [8, 4, 512, 64], F32)
    lxp = I("lxp", [2, 64, 259 + S + 3], F32)
    OT = g.out("OT", [768, TT], F32)
    LY = g.out("LY", [2, 64, CT + S], F32)
    NK = (S + CT) // 128
    ones = sb("p2_ones", [128, 64], F32)
    o.ms(ones[:, :], 1.0, [ones.k])
    TK = Trk()

    ps_s = [ps(f"p2_s{i}", [128, 512], F32) for i in range(3)]
    ps_o = [ps(f"p2_o{i}", [128, 512], F32) for i in range(2)]
    ps_b = ps("p2_b", [128, 512], F32)
    ps_g = [ps(f"p2_g{i}", [128, 512], F32) for i in range(2)]
    pbuf = [sb(f"p2_p{i}", [128, 512], BF16) for i in range(3)]
    rsb = sb("p2_rs", [128, 512], F32); osb = sb("p2_osb", [64, 512], F32); onb = [sb(f"p2_on{i}", [64, 512], F32) for i in range(2)]
    cnt = [0]

    def finalize(po, n, row0, col0):
        o.rcp(rsb[64:65, 0:n], po[64:65, 0:n], [po.k], [rsb.k])
        o.mm(ps_b[0:64, 0:n], ones[64:65, 0:64], rsb[64:65, 0:n], True, True, [ones.k, rsb.k], [ps_b.k])
        o.cp(osb[:, 0:n], po[0:64, 0:n], [po.k], [osb.k], eng="act" if False else "dve")
        B = onb[cnt[0] % 2]; cnt[0] += 1
        o.tt(B[:, 0:n], osb[:, 0:n], ps_b[0:64, 0:n], ALU.mult, [osb.k, ps_b.k], [B.k])
        o.dma(OT[row0:row0 + 64, col0:col0 + n], B[:, 0:n], [B.k], [DK(OT)])

    def dense_head(QT, dk, KT, VA, vsl, scale, row0, kts_lat, kts_ctx):
        it = 0
        for (q0, n, kts) in [(c * 512, 512, kts_lat) for c in range(4)] + [(TL, CT, kts_ctx)]:
            po = ps_o[(q0 // 512) % 2]
            for ki, kt in enumerate(kts):
                pS = ps_s[it % 3]; P = pbuf[it % 3]; it += 1
                o.mm(pS[:, 0:n], KT[0:dk, kt * 128:(kt + 1) * 128], QT[0:dk, q0:q0 + n], True, True, [KT.k, QT.k], [pS.k])
                o.act(P[:, 0:n], pS[:, 0:n], AF.Exp, [pS.k], [P.k], scale=scale)
                o.mm(po[0:65, 0:n], vsl(kt), P[:, 0:n], ki == 0, ki == len(kts) - 1, [VA.k, P.k], [po.k])
            finalize(po, n, row0, q0)

    with contextlib.ExitStack() as ss:
        KT = sb("g_KT", [64, NK * 128], BF16, ss); VA = sb("g_VA", [128, NK, 130], BF16, ss)
        QT = [sb(f"g_QT{i}", [64, TT], BF16, ss) for i in range(2)]
        o.dma(VA[:, :, :], dap(gv, 0, [[130, 128], [128 * 130, NK], [1, 130]]), [], [VA.k])
        for h in range(4):
            kvh = h // 2
            if h % 2 == 0:
                o.dma(KT[:, :], gk[kvh * 64:(kvh + 1) * 64, :], [], [KT.k])
            Q = QT[h % 2]
            o.dma(Q[:, :], gq[h * 64:(h + 1) * 64, :], [], [Q.k])
            dense_head(Q, 64, KT, VA, lambda kt, kvh=kvh: VA[:, kt, kvh * 65:(kvh + 1) * 65], 0.125, 256 + h * 64,
                       list(range(NK)), [NK - 2, NK - 1])
    with contextlib.ExitStack() as ss:
        KT = sb("m_KT", [96, NK * 128], BF16, ss); VA = sb("m_VA", [128, NK, 260], BF16, ss)
        QT = [sb(f"m_QT{i}", [96, TT], BF16, ss) for i in range(2)]
        o.dma(VA[:, :, :], dap(mv, 0, [[260, 128], [128 * 260, NK], [1, 260]]), [], [VA.k])
        for h in range(4):
            o.dma(KT[:, :], mk[h * 96:(h + 1) * 96, :], [], [KT.k])
            Q = QT[h % 2]
            o.dma(Q[:, :], mq[h * 96:(h + 1) * 96, :], [], [Q.k])
            dense_head(Q, 96, KT, VA, lambda kt, h=h: VA[:, kt, h * 65:(h + 1) * 65], 96 ** -0.5, 512 + h * 64,
                       list(range(NK)), [NK - 2, NK - 1])
    with contextlib.ExitStack() as ss:
        QT = sb("n_QT", [64, 4, TT], BF16, ss)
        KC = sb("n_KC", [64, 4, CT], BF16, ss); VC = sb("n_VC", [128, 2, 260], BF16, ss)
        E = sb("n_E", [128, 8, 4, 4, 64], BF16, ss); Ef = sb("n_Ef", [128, 4, 4, 64], F32, ss)
        KR = [sb(f"n_KR{i}", [64, 4, 512], BF16, ss) for i in range(2)]
        VR = [sb(f"n_VR{i}", [128, 4, 260], BF16, ss) for i in range(2)]
        Pn = [sb(f"n_P{i}", [128, 384], BF16, ss) for i in range(2)]; Pf = sb("n_Pf", [128, 384], F32, ss)
        for h in range(4):
            o.dma(QT[:, h, :], nq[h * 64:(h + 1) * 64, :], [], [QT.k])
            o.dma(KC[:, h, :], nkc[h * 64:(h + 1) * 64, :], [], [KC.k])
        o.dma(VC[:, :, :], dap(nvc, 0, [[260, 128], [128 * 260, 2], [1, 260]]), [], [VC.k])
        for v in range(8):
            for h in range(4):
                o.dma(Ef[:, h, :, :], dap(eb, (v * 4 + h) * 512 * 64, [[64, 128], [128 * 64, 4], [1, 64]]), [], [Ef.k])
            o.act(E[:, v, :, :, :], Ef[:, :, :, :], AF.Exp, [Ef.k], [E.k])
        it = 0
        for h in range(4):
            for lr in range(32):
                if h == 0 or True:
                    pass
            pass
        for rg in range(4):
            for h in range(4):
                pass
        for rg in range(4):
            for lr8 in range(8):
                lr = rg * 8 + lr8
                K_ = KR[lr % 2]; V_ = VR[lr % 2]
                o.dma(K_[:, :, :], dap(nkt, lr * 256 * 512, [[512, 64], [64 * 512, 4], [1, 512]]), [], [K_.k])
                o.dma(V_[:, :, :], dap(nv, lr * 512 * 260, [[260, 128], [128 * 260, 4], [1, 260]]), [], [V_.k])
                for h in range(4):
                    po = (ps_o + ps_g)[h]
                    pS = ps_s[it % 3]; P = Pn[it % 2]; it += 1
                    q = QT[:, h, lr * 64:(lr + 1) * 64]
                    for kt in range(4):
                        o.mm(pS[:, kt * 64:(kt + 1) * 64], K_[:, h, kt * 128:(kt + 1) * 128], q, True, True, [K_.k, QT.k], [pS.k])
                    for kt in range(2):
                        o.mm(pS[:, 256 + kt * 64:256 + (kt + 1) * 64], KC[:, h, kt * 128:(kt + 1) * 128], q, True, True, [KC.k, QT.k], [pS.k])
                    o.act(Pf[:, 0:256], pS[:, 0:256], AF.Exp, [pS.k], [Pf.k], scale=0.125)
                    o.act(P[:, 256:384], pS[:, 256:384], AF.Exp, [pS.k], [P.k], scale=0.125)
                    o.tt(P[:, 0:256], Pf[:, 0:256], E[:, na_var(lr), h, :, :].rearrange("p a b -> p (a b)"), ALU.mult, [Pf.k, E.k], [P.k])
                    for kt in range(6):
                        lhs = V_[:, kt, h * 65:(h + 1) * 65] if kt < 4 else VC[:, kt - 4, h * 65:(h + 1) * 65]
                        o.mm(po[0:65, lr8 * 64:(lr8 + 1) * 64], lhs, P[:, kt * 64:(kt + 1) * 64], kt == 0, kt == 5, [V_.k, VC.k, P.k], [po.k])
            for h in range(4):
                finalize((ps_o + ps_g)[h], 512, h * 64, rg * 512)
        for h in range(4):
            po = (ps_o + ps_g)[h]
            pS = ps_s[it % 3]; it += 1
            P2 = pbuf[h % 3]
            for kt in range(2):
                o.mm(pS[:, kt * 256:(kt + 1) * 256], KC[:, h, kt * 128:(kt + 1) * 128], QT[:, h, TL:TT], True, True, [KC.k, QT.k], [pS.k])
            o.act(P2[:, 0:512], pS[:, 0:512], AF.Exp, [pS.k], [P2.k], scale=0.125)
            for kt in range(2):
                o.mm(po[0:65, 0:256], VC[:, kt, h * 65:(h + 1) * 65], P2[:, kt * 256:(kt + 1) * 256], kt == 0, kt == 1, [VC.k, P2.k], [po.k])
            finalize(po, 256, h * 64, TL)
    with contextlib.ExitStack() as ss:
        W = sb("l_W", [64, 2, 2, 64], F32, ss)
        cpl = sb("l_cp", [64, 16], F32, ss)
        cneg = sb("l_cneg", [64, 2], F32, ss); tmpc = sb("l_tmpc", [64, 2], F32, ss)
        xp = sb("l_xp", [64, 2051], F32, ss); xc_ = sb("l_xc", [64, 2048], F32, ss)
        ra = sb("l_ra", [64, 2048], F32, ss); ri = sb("l_ri", [64, 2048], F32, ss)
        a2 = sb("l_a2", [64, 2048], F32, ss); hh = sb("l_h", [64, 2048], F32, ss)
        hprev = sb("l_hp", [64, 1], F32, ss)
        o.dma(W[:, :, :, :], iv("lru_w", [[64, 64], [64 * 64, 4], [1, 64]]).rearrange("k (d t) m -> k d t m", t=2), [], [W.k])
        o.dma(cpl[:, :], iv("lru_cp", [[16, 64], [1, 16]]), [], [cpl.k])
        o.act(tmpc[:, :], cpl[:, 9:11], AF.Exp, [cpl.k], [tmpc.k], scale=-1.0)
        o.act(tmpc[:, :], tmpc[:, :], AF.Ln, [tmpc.k], [tmpc.k], bias=1.0)
        o.ts(cneg[:, :], tmpc[:, :], -8.0, None, ALU.mult, ALU.bypass, [tmpc.k], [cneg.k])
        for dr in range(2):
            first = True
            for (p0, n, o0) in [(0, CT, 0)] + [(259 + c * 2048, 2048, CT + c * 2048) for c in range(8)]:
                o.dma(xp[:, 0:n + 3], dap(lxp, dr * 64 * (262 + S) + p0, [[262 + S, 64], [1, n + 3]]), [], [xp.k])
                for k in range(4):
                    off = k if dr == 0 else 3 - k
                    if k == 0:
                        o.ts(xc_[:, 0:n], xp[:, off:off + n], cpl[:, 0:1], cpl[:, 4:5], ALU.mult, ALU.add, [xp.k, cpl.k], [xc_.k])
                    else:
                        o.stt(xc_[:, 0:n], xp[:, off:off + n], cpl[:, k:k + 1], xc_[:, 0:n], ALU.mult, ALU.add, [xp.k, cpl.k, xc_.k], [xc_.k])
                for c in range(0, n, 512):
                    cn = min(512, n - c)
                    for ty, dst in ((0, ra), (1, ri)):
                        pg = ps_g[ty]
                        o.mm(pg[0:64, 0:cn], W[:, dr, ty, :], xc_[:, c:c + cn], True, True, [W.k, xc_.k], [pg.k])
                        bcol = cpl[:, 5 + 2 * ty + dr:6 + 2 * ty + dr]
                        o.act(dst[:, c:c + cn], pg[0:64, 0:cn], AF.Sigmoid, [pg.k, cpl.k], [dst.k], bias=bcol)
                o.act(ra[:, 0:n], ra[:, 0:n], AF.Exp, [ra.k, cneg.k], [ra.k], scale=cneg[:, dr:dr + 1])
                o.tt(a2[:, 0:n], ra[:, 0:n], ra[:, 0:n], ALU.mult, [ra.k], [a2.k])
                o.act(a2[:, 0:n], a2[:, 0:n], AF.Sqrt, [a2.k], [a2.k], scale=-1.0, bias=1.0)
                o.tt(ri[:, 0:n], ri[:, 0:n], xc_[:, 0:n], ALU.mult, [ri.k, xc_.k], [ri.k])
                o.tt(ri[:, 0:n], ri[:, 0:n], a2[:, 0:n], ALU.mult, [ri.k, a2.k], [ri.k])
                init = 0.0 if first else hprev[:, 0:1]
                kb.op("dve", lambda e, n=n, init=init: e.tensor_tensor_scan(out=hh[:, 0:n], data0=ra[:, 0:n], data1=ri[:, 0:n],
                                                                            initial=init, op0=ALU.mult, op1=ALU.add),
                      [ra.k, ri.k, hprev.k], [hh.k])
                o.cp(hprev[:, 0:1], hh[:, n - 1:n], [hh.k], [hprev.k])
                o.dma(LY[dr, :, o0:o0 + n], hh[:, 0:n], [hh.k], [DK(LY)])
                first = False


def _ebias_table(rpb, j):
    out = np.full((8, 4, 8, 64, 64), -30000.0, np.float32)
    c = np.arange(64); c0 = np.clip(c - 8, 0, 48)
    kc = np.arange(64)
    valid = (kc[:, None] >= c0[None, :]) & (kc[:, None] < c0[None, :] + 16)
    relc = np.clip(kc[:, None] - c[None, :] + 15, 0, 30)
    lrs = [4, 0, 1, 2, 3, 29, 30, 31]
    for v, lr in enumerate(lrs):
        r = 32 * j + lr; r0 = min(max(r - 4, 0), 248); dl = r - r0
        for i in range(8):
            dr_ = i - dl + 7
            tab = rpb[:, dr_, :][:, relc]
            out[v, :, i] = np.where(valid[None], tab, np.float32(-30000.0))
    return out.reshape(8, 4, 512, 64)


def host_p2(inp, l, p1):
    f = lambda n: [np.asarray(r[n]) for r in p1]
    cat = lambda n, ax: np.concatenate(f(n), axis=ax)
    gk_all = np.concatenate([cat("GK_src", 1), p1[0]["GKC"]], axis=1)
    gv_all = np.concatenate([cat("GV_src", 0), p1[0]["GVC"]], axis=0)
    mkn = np.concatenate([cat("MKN_src", 1), p1[0]["MKNC"]], axis=1)
    mkr = np.concatenate([cat("MKR_src", 1), p1[0]["MKRC"]], axis=1)
    mk_all = np.concatenate([np.concatenate([mkn[h * 64:(h + 1) * 64], mkr], axis=0) for h in range(4)], axis=0)
    mv_all = np.concatenate([cat("MV_src", 0), p1[0]["MVC"]], axis=0)
    nak_all = cat("NAK_src", 0); nav_all = cat("NAV_src", 0)
    lx_l = cat("LX", 1)
    LX = [np.asarray(r["LX"]) for r in p1]
    lx_lat = np.concatenate([a[:, :TL] for a in LX], axis=1)
    lx_ctx = LX[0][:, TL:]
    maps = []
    for j in range(NCORE):
        m = {}
        m["gq"] = p1[j]["GQ"]; m["gk"] = gk_all; m["gv"] = gv_all
        m["mq"] = p1[j]["MQ"]; m["mk"] = mk_all; m["mv"] = mv_all
        m["nq"] = p1[j]["NQ"]
        nkt = np.empty((32, 256, 512), nak_all.dtype); nvv = np.empty((32, 512, 260), nav_all.dtype)
        for lr in range(32):
            r = 32 * j + lr; r0 = min(max(r - 4, 0), 248)
            nkt[lr] = nak_all[r0 * 64:r0 * 64 + 512].T
            nvv[lr] = nav_all[r0 * 64:r0 * 64 + 512]
        m["nkt"] = nkt; m["nv"] = nvv
        m["nkc"] = np.ascontiguousarray(np.asarray(p1[0]["NAKC"]).T); m["nvc"] = p1[0]["NAVC"]
        m["ebias"] = _ebias_table(np.asarray(inp["na_rpb"][l], np.float32), j)
        gb = j % 4
        def padded(a):
            return np.concatenate([np.zeros((64, 1), np.float32), a, np.zeros((64, 2), np.float32)], axis=1)
        pc = padded(lx_ctx[gb * 64:(gb + 1) * 64]); pl = padded(lx_lat[gb * 64:(gb + 1) * 64])
        m["lxp"] = np.ascontiguousarray(np.stack([np.concatenate([pc, pl], axis=1),
                                                  np.concatenate([pc[:, ::-1], pl[:, ::-1]], axis=1)]))
        pk = Packer()
        w = np.stack([(inp["lru_wa"] if t == 0 else inp["lru_wi"])[l][d_][gb] for d_ in range(2) for t in range(2)])
        pk.add("lru_w", w)
        cpl = np.zeros((64, 16), np.float32)
        sl = slice(gb * 64, (gb + 1) * 64)
        cpl[:, 0:4] = inp["lru_conv_w"][l].reshape(4, 256)[:, sl].T
        cpl[:, 4] = inp["lru_conv_b"][l][sl]
        cpl[:, 5:7] = inp["lru_ba"][l][:, sl].T; cpl[:, 7:9] = inp["lru_bi"][l][:, sl].T; cpl[:, 9:11] = inp["lru_lam"][l][:, sl].T
        pk.add("lru_cp", cpl)
        arr, irows = pk.finish_single()
        m["inp"] = arr
        maps.append({k: np.ascontiguousarray(v) for k, v in m.items()})
    if "p2" not in _CACHE: _CACHE["p2"] = build(emit_p2, pk.items, irows)
    return run(_CACHE["p2"], maps)


def emit_p3(g):
    nc, kb, o, sb, ps, DK, iv = g.nc, g.kb, g.o, g.sb, g.ps, g.DK, g.iv
    d = g.dram
    I = lambda n, s, dt: d(n, s, dt, "ExternalInput")
    OTi = I("ot", [768, TT], F32); LYi = I("ly", [2, 256, TT], F32); LGi = I("lg", [256, TT], F32)
    X1 = g.out("X1", [TT, D], F32); H2 = g.out("H2", [TT, D], BF16); RT = g.out("RT", [TT, 8], F32)
    load_consts(g, ["ident", "ones128"])
    wout = sb("wout", [128, 8, 1024], BF16); stg = [sb(f"stg{i}", [128, 1024], F32) for i in range(2)]
    for k in range(8):
        s_ = stg[k % 2]
        o.dma(s_[:, :], iv("w_out", [[1024, 128], [1, 1024]], k * 128 * 1024), [], [s_.k])
        o.cp(wout[:, k, :], s_[:, :], [s_.k], [wout.k], eng="pool")
    rw = sb("rw", [128, 8, 32], F32); rb = sb("rb", [128, 32], F32); cpk = sb("cpk", [128, NCOL], F32)
    o.dma(rw[:, :, :], iv("router_w", [[32, 128], [128 * 32, 8], [1, 32]]), [], [rw.k])
    o.dma(rb[:, :], iv("router_b", [[0, 128], [1, 32]]), [], [rb.k])
    o.dma(cpk[:, :], iv("cp", [[NCOL, 128], [1, NCOL]]), [], [cpk.k])
    GT1 = [sb(f"GT1{w}", [128, 1024], F32) for w in range(2)]; G2 = [sb(f"G2{w}", [128, 1024], F32) for w in range(2)]
    SH2 = [sb(f"SH2{w}", [128, 1024], F32) for w in range(2)]
    tA = sb("tA", [128, 1024], F32); tB = sb("tB", [128, 1024], F32)
    mv_ = lambda w, v: iv("modv", [[0, 128], [1, 1024]], (w * 6 + v) * 1024)
    o.dma(tA[:, :], iv("norm2_g", [[0, 128], [1, 1024]]), [], [tA.k])
    for w in range(2):
        o.dma(GT1[w][:, :], mv_(w, 2), [], [GT1[w].k])
        o.dma(tB[:, :], mv_(w, 4), [], [tB.k])
        o.stt(G2[w][:, :], tB[:, :], 1.0, tA[:, :], ALU.add, ALU.mult, [tB.k, tA.k], [G2[w].k])
        o.dma(SH2[w][:, :], mv_(w, 3), [], [SH2[w].k])
    O_ = sb("O_", [128, 8, 512], F32); yf = sb("yf", [128, 512], F32); yb = sb("yb", [128, 512], F32); lgt = sb("lgt", [128, 512], F32)
    sq = [sb(f"sq{i}", [128, 512], F32) for i in range(2)]; srt = sb("srt", [128, 512], F32); rs = sb("rs", [128, 512], F32)
    yn = sb("yn", [128, 8, 512], BF16)
    xt = [sb(f"xt{i}", [128, 1024], F32) for i in range(2)]; x1 = [sb(f"x1{i}", [128, 1024], F32) for i in range(2)]
    h2 = [sb(f"h2{i}", [128, 1024], F32) for i in range(2)]; h2b = [sb(f"h2b{i}", [128, 1024], BF16) for i in range(2)]
    h2T = sb("h2T", [128, 8, 128], F32)
    ssq = sb("ssq", [128, 2], F32); srt1 = sb("srt1", [128, 2], F32); rs1 = sb("rs1", [128, 2], F32)
    lg32 = sb("lg32", [128, 32], F32); v8 = sb("v8", [128, 8], F32); i8 = sb("i8", [128, 8], U32)
    nv0 = sb("nv0", [128, 1], F32); se = sb("se", [128, 1], F32); rse = sb("rse", [128, 1], F32); e4 = sb("e4", [128, 4], F32)
    rtt = [sb(f"rtt{i}", [128, 8], F32) for i in range(2)]
    pst = ps("pst", [128, 512], F32); py = [ps(f"py{i}", [128, 512], F32) for i in range(2)]
    pT = ps("pT", [128, 8, 128], F32); plog = ps("plog", [128, 512], F32)
    XK = Trk()
    for (t0, n) in TCH:
        for c in range(6):
            o.dma(O_[:, c, 0:n], OTi[c * 128:(c + 1) * 128, t0:t0 + n], [], [O_.k])
        for c in range(2):
            o.dma(yf[:, 0:n], LYi[0, c * 128:(c + 1) * 128, t0:t0 + n], [], [yf.k])
            o.dma(yb[:, 0:n], LYi[1, c * 128:(c + 1) * 128, t0:t0 + n], [], [yb.k])
            o.dma(lgt[:, 0:n], LGi[c * 128:(c + 1) * 128, t0:t0 + n], [], [lgt.k])
            o.tt(yf[:, 0:n], yf[:, 0:n], yb[:, 0:n], ALU.add, [yf.k, yb.k], [yf.k])
            o.act(lgt[:, 0:n], lgt[:, 0:n], AF.Gelu, [lgt.k], [lgt.k])
            o.tt(O_[:, 6 + c, 0:n], yf[:, 0:n], lgt[:, 0:n], ALU.mult, [yf.k, lgt.k], [O_.k])
        for gi in range(4):
            for c in range(2):
                o.act(sq[c][:, 0:n], O_[:, 2 * gi + c, 0:n], AF.Square, [O_.k], [sq[c].k])
            for c in range(2):
                o.mm(pst[:, 0:n], g.ones128[:, :], sq[c][:, 0:n], c == 0, c == 1, [g.ones128.k, sq[c].k], [pst.k])
            o.act(srt[:, 0:n], pst[:, 0:n], AF.Sqrt, [pst.k], [srt.k], scale=1.0 / 256, bias=EPS)
            o.rcp(rs[:, 0:n], srt[:, 0:n], [srt.k], [rs.k])
            for c in range(2):
                cc = 2 * gi + c
                gcol = cpk[:, CP["grp_g"][0] + cc:CP["grp_g"][0] + cc + 1]
                o.stt(yn[:, cc, 0:n], O_[:, cc, 0:n], gcol, rs[:, 0:n], ALU.mult, ALU.mult, [O_.k, rs.k, cpk.k], [yn.k])
        for sub in range(n // 128):
            t = t0 // 128 + sub; w = 0 if t < NTL else 1; i = t % 2
            for hf in range(2):
                for c in range(8):
                    o.mm(py[hf][:, 0:512], yn[:, c, sub * 128:(sub + 1) * 128], wout[:, c, hf * 512:(hf + 1) * 512], c == 0, c == 7,
                         [yn.k, wout.k], [py[hf].k])
            o.dma(xt[i][:, :], iv('x', [[D, 128], [1, D]], t * 128 * D) if t < NTL else iv('ctx', [[D, 128], [1, D]], (t - NTL) * 128 * D), [XK], [xt[i].k])
            for hf in range(2):
                o.tt(tA[:, hf * 512:(hf + 1) * 512], py[hf][:, 0:512], GT1[w][:, hf * 512:(hf + 1) * 512], ALU.mult, [py[hf].k, GT1[w].k], [tA.k])
            o.tt(x1[i][:, :], xt[i][:, :], tA[:, :], ALU.add, [xt[i].k, tA.k], [x1[i].k])
            o.dma(X1[t * 128:(t + 1) * 128, :], x1[i][:, :], [x1[i].k], [DK(X1)])
            o.act(tB[:, :], x1[i][:, :], AF.Square, [x1[i].k], [tB.k, ssq.k], accum_out=ssq[:, i:i + 1])
            o.act(srt1[:, i:i + 1], ssq[:, i:i + 1], AF.Sqrt, [ssq.k], [srt1.k], scale=1.0 / D, bias=EPS)
            o.rcp(rs1[:, i:i + 1], srt1[:, i:i + 1], [srt1.k], [rs1.k])
            o.stt(tB[:, :], x1[i][:, :], rs1[:, i:i + 1], G2[w][:, :], ALU.mult, ALU.mult, [x1[i].k, rs1.k, G2[w].k], [tB.k])
            o.tt(h2[i][:, :], tB[:, :], SH2[w][:, :], ALU.add, [tB.k, SH2[w].k], [h2[i].k])
            o.cp(h2b[i][:, :], h2[i][:, :], [h2[i].k], [h2b[i].k], eng="pool")
            o.dma(H2[t * 128:(t + 1) * 128, :], h2b[i][:, :], [h2b[i].k], [DK(H2)])
            for k in range(8):
                o.tr(pT[:, k, :], h2[i][:, k * 128:(k + 1) * 128], g.ident_f[:, :], [h2[i].k, g.ident_f.k], [pT.k])
            o.cp(h2T[:, :, :], pT[:, :, :], [pT.k], [h2T.k])
            for k in range(8):
                o.mm(plog[:, 0:32], h2T[:, k, :], rw[:, k, :], k == 0, k == 7, [h2T.k, rw.k], [plog.k])
            o.tt(lg32[:, :], plog[:, 0:32], rb[:, :], ALU.add, [plog.k, rb.k], [lg32.k])
            kb.op("dve", lambda e: e.max(out=v8[:, :], in_=lg32[:, :]), [lg32.k], [v8.k])
            kb.op("dve", lambda e: e.max_index(out=i8[:, :], in_max=v8[:, :], in_values=lg32[:, :]), [v8.k, lg32.k], [i8.k])
            o.ts(nv0[:, :], v8[:, 0:1], -1.0, None, ALU.mult, ALU.bypass, [v8.k], [nv0.k])
            o.act(e4[:, :], v8[:, 0:4], AF.Exp, [v8.k, nv0.k], [e4.k, se.k], bias=nv0[:, 0:1], accum_out=se[:, 0:1])
            o.rcp(rse[:, :], se[:, :], [se.k], [rse.k])
            R = rtt[i]
            o.ts(R[:, 4:8], e4[:, :], rse[:, 0:1], None, ALU.mult, ALU.bypass, [e4.k, rse.k], [R.k])
            o.cp(R[:, 0:4], i8[:, 0:4], [i8.k], [R.k])
            o.dma(RT[t * 128:(t + 1) * 128, :], R[:, :], [R.k], [DK(RT)])


NTOK = S + CT
def emit_p4(g):
    nc, kb, o, sb, ps, DK, iv = g.nc, g.kb, g.o, g.sb, g.ps, g.DK, g.iv
    d = g.dram
    I = lambda n, s, dt: d(n, s, dt, "ExternalInput")
    H2T = I("h2t", [D, NTOK], BF16); RTi = I("rt", [NTOK, 8], F32)
    WGU = I("wgu", [ELOC, D, 2048], F32); WD = I("wd", [ELOC, D, D], F32)
    FP = g.out("FP", [NTOK, D], BF16)
    wgu2 = [sb(f"wgu_b{i}", [128, 8, 2048], BF16) for i in range(2)]; wd2 = [sb(f"wd_b{i}", [128, 8, 1024], BF16) for i in range(2)]
    bgu = sb("bgu", [128, 16], F32); bdn = sb("bdn", [128, 1024], F32); eid = sb("eid", [128, 4], F32)
    hT = [sb(f"hT{i}", [128, 8, 512], BF16) for i in range(2)]
    actT = sb("actT", [128, 8, 512], BF16)
    xg = [sb(f"xg{i}", [128, 512], F32) for i in range(2)]; sg = [sb(f"sg{i}", [128, 512], F32) for i in range(2)]
    xl = [sb(f"xl{i}", [128, 512], F32) for i in range(2)]
    rt = sb("rt_sb", [128, 4, 8], F32); eq = sb("eq", [128, 4, 4], F32); gate = sb("gate", [128, 4, 1], F32)
    ft = [sb(f"ft{i}", [128, 1024], BF16) for i in range(2)]; yt = [sb(f"yt{i}", [128, 1024], F32) for i in range(2)]
    ytb = [sb(f"ytb{i}", [128, 1024], BF16) for i in range(2)]
    pg = [ps(f"pg{i}", [128, 512], F32) for i in range(4)]; pd = [ps(f"pd{i}", [128, 512], F32) for i in range(4)]
    o.dma(eid[:, :], iv("eid", [[0, 128], [1, 4]]), [], [eid.k])
    chunks = [(c * 512, 512) for c in range(NTOK // 512)] + [(NTOK // 512 * 512, NTOK % 512)]
    for e in range(ELOC):
        wgu = wgu2[e % 2]; wd = wd2[e % 2]
        for k in range(8):
            o.dma(wgu[:, k, :], WGU[e, k * 128:(k + 1) * 128, :], [], [wgu.k], q="pool")
            o.dma(wd[:, k, :], WD[e, k * 128:(k + 1) * 128, :], [], [wd.k], q="pool")
        o.dma(bgu[:, :], iv("bgu", [[16, 128], [1, 16]], e * 2048), [], [bgu.k])
        o.dma(bdn[:, :], iv("bd", [[0, 128], [1, 1024]], e * 1024), [], [bdn.k])
        for ci, (t0, n) in enumerate(chunks):
            H = hT[ci % 2]
            o.dma(H[:, :, 0:n], dap(H2T, t0, [[NTOK, 128], [128 * NTOK, 8], [1, n]]), [], [H.k])
            nsub = n // 128
            o.dma(rt[:, 0:nsub, :], dap(RTi, t0 * 8, [[8, 128], [128 * 8, nsub], [1, 8]]), [], [rt.k])
            o.ts(eq[:, 0:nsub, :], rt[:, 0:nsub, 0:4], eid[:, e:e + 1], None, ALU.is_equal, ALU.bypass, [rt.k, eid.k], [eq.k])
            o.tt(eq[:, 0:nsub, :], eq[:, 0:nsub, :], rt[:, 0:nsub, 4:8], ALU.mult, [eq.k, rt.k], [eq.k])
            o.red(gate[:, 0:nsub, 0], eq[:, 0:nsub, :], ALU.add, [eq.k], [gate.k])
            for c in range(8):
                i = c % 2
                for half, P_ in ((0, pg[2 * i]), (1, pg[2 * i + 1])):
                    cc = c + 8 * half
                    for k in range(8):
                        o.mm(P_[:, 0:n], wgu[:, k, cc * 128:(cc + 1) * 128], H[:, k, 0:n], k == 0, k == 7, [wgu.k, H.k], [P_.k])
                o.ts(xg[i][:, 0:n], pg[2 * i][:, 0:n], bgu[:, c:c + 1], 7.0, ALU.add, ALU.min, [pg[2 * i].k, bgu.k], [xg[i].k])
                o.act(sg[i][:, 0:n], xg[i][:, 0:n], AF.Sigmoid, [xg[i].k], [sg[i].k], scale=1.702)
                o.ts(xl[i][:, 0:n], pg[2 * i + 1][:, 0:n], bgu[:, 8 + c:9 + c], 7.0, ALU.add, ALU.min, [pg[2 * i + 1].k, bgu.k], [xl[i].k])
                o.ts(xl[i][:, 0:n], xl[i][:, 0:n], -7.0, 1.0, ALU.max, ALU.add, [xl[i].k], [xl[i].k])
                o.tt(xg[i][:, 0:n], xg[i][:, 0:n], sg[i][:, 0:n], ALU.mult, [xg[i].k, sg[i].k], [xg[i].k])
                o.tt(actT[:, c, 0:n], xg[i][:, 0:n], xl[i][:, 0:n], ALU.mult, [xg[i].k, xl[i].k], [actT.k])
            for sub in range(nsub):
                i = sub % 2
                r0 = t0 + sub * 128
                if e > 0:
                    o.dma(ft[i][:, :], FP[r0:r0 + 128, :], [DK(FP)], [ft[i].k])
                for hf in range(2):
                    P_ = pd[2 * i + hf]
                    for k in range(8):
                        o.mm(P_[:, 0:512], actT[:, k, sub * 128:(sub + 1) * 128], wd[:, k, hf * 512:(hf + 1) * 512], k == 0, k == 7,
                             [actT.k, wd.k], [P_.k])
                    o.tt(yt[i][:, hf * 512:(hf + 1) * 512], P_[:, 0:512], bdn[:, hf * 512:(hf + 1) * 512], ALU.add, [P_.k, bdn.k], [yt[i].k])
                if e > 0:
                    o.stt(ytb[i][:, :], yt[i][:, :], gate[:, sub, 0:1], ft[i][:, :], ALU.mult, ALU.add, [yt[i].k, gate.k, ft[i].k], [ytb[i].k])
                else:
                    o.ts(ytb[i][:, :], yt[i][:, :], gate[:, sub, 0:1], None, ALU.mult, ALU.bypass, [yt[i].k, gate.k], [ytb[i].k])
                o.dma(FP[r0:r0 + 128, :], ytb[i][:, :], [ytb[i].k], [DK(FP)])


def emit_p5(g):
    nc, kb, o, sb, ps, DK, iv = g.nc, g.kb, g.o, g.sb, g.ps, g.DK, g.iv
    d = g.dram
    FPi = d("fp", [NCORE, TT, D], BF16, "ExternalInput"); X1i = d("x1", [TT, D], F32, "ExternalInput")
    X2 = g.out("X2", [TT, D], F32)
    GT2 = [sb(f"GT2{w}", [128, 1024], F32) for w in range(2)]
    for w in range(2):
        o.dma(GT2[w][:, :], iv("modv", [[0, 128], [1, 1024]], (w * 6 + 5) * 1024), [], [GT2[w].k])
    fin = [sb(f"fin{i}", [128, 1024], BF16) for i in range(4)]; acc = [sb(f"acc{i}", [128, 1024], F32) for i in range(2)]
    xt = [sb(f"xt{i}", [128, 1024], F32) for i in range(2)]
    for t in range(NT):
        w = 0 if t < NTL else 1; i = t % 2; A = acc[i]
        for c in range(NCORE):
            F_ = fin[c % 4]
            o.dma(F_[:, :], FPi[c, t * 128:(t + 1) * 128, :], [], [F_.k])
            if c == 0:
                o.cp(A[:, :], F_[:, :], [F_.k], [A.k])
            else:
                o.tt(A[:, :], A[:, :], F_[:, :], ALU.add, [A.k, F_.k], [A.k])
        o.dma(xt[i][:, :], X1i[t * 128:(t + 1) * 128, :], [], [xt[i].k])
        o.tt(A[:, :], A[:, :], GT2[w][:, :], ALU.mult, [A.k, GT2[w].k], [A.k])
        o.tt(A[:, :], A[:, :], xt[i][:, :], ALU.add, [A.k, xt[i].k], [A.k])
        o.dma(X2[t * 128:(t + 1) * 128, :], A[:, :], [A.k], [DK(X2)])


def host_p3(inp, l, x, xc, mod, p1, p2):
    maps = []
    lys = [np.asarray(p2[gb]["LY"], np.float32) for gb in range(4)]
    for j in range(NCORE):
        m = {"ot": np.asarray(p2[j]["OT"]), "lg": np.asarray(p1[j]["LG"])}
        ly = np.empty((2, 256, TT), np.float32)
        for gb in range(4):
            L_ = lys[gb]
            ly[0, gb * 64:(gb + 1) * 64, :TL] = L_[0][:, CT + j * TL:CT + (j + 1) * TL]
            ly[0, gb * 64:(gb + 1) * 64, TL:] = L_[0][:, :CT]
            ly[1, gb * 64:(gb + 1) * 64, :TL] = L_[1][:, CT:][:, ::-1][:, j * TL:(j + 1) * TL]
            ly[1, gb * 64:(gb + 1) * 64, TL:] = L_[1][:, :CT][:, ::-1]
        m["ly"] = ly
        pk = Packer()
        add_consts(pk)
        pk.add("w_out", inp["w_out"][l]); pk.add("router_w", inp["router_w"][l]); pk.add("router_b", inp["router_b"][l])
        pk.add("norm2_g", inp["norm2_g"][l]); pk.add("cp", make_cp(inp, l))
        pk.add("x", x[j * TL:(j + 1) * TL]); pk.add("ctx", xc)
        pk.add("modv", mod[:, l, :].reshape(2, 6, 1024))
        arr, irows = pk.finish_single()
        m["inp"] = arr
        maps.append({k: np.ascontiguousarray(v) for k, v in m.items()})
    if "p3" not in _CACHE: _CACHE["p3"] = build(emit_p3, pk.items, irows)
    return run(_CACHE["p3"], maps)


def host_p4(inp, l, p3):
    h2 = np.concatenate([np.asarray(r["H2"])[:TL] for r in p3] + [np.asarray(p3[0]["H2"])[TL:]], axis=0)
    h2t = np.ascontiguousarray(h2.T)
    rt = np.ascontiguousarray(np.concatenate([np.asarray(r["RT"])[:TL] for r in p3] + [np.asarray(p3[0]["RT"])[TL:]], axis=0))
    maps = []
    for j in range(NCORE):
        es = slice(j * ELOC, (j + 1) * ELOC)
        pk = Packer()
        pk.add("eid", np.arange(j * ELOC, (j + 1) * ELOC, dtype=np.float32))
        pk.add("bgu", np.stack([inp["exp_b_gu"][l][e].reshape(16, 128).T for e in range(j * ELOC, (j + 1) * ELOC)]))
        pk.add("bd", inp["exp_b_down"][l][es])
        arr, irows = pk.finish_single()
        maps.append({"inp": arr, "h2t": h2t, "rt": rt,
                     "wgu": np.ascontiguousarray(inp["exp_w_gu"][l][es], dtype=np.float32),
                     "wd": np.ascontiguousarray(inp["exp_w_down"][l][es], dtype=np.float32)})
    if "p4" not in _CACHE: _CACHE["p4"] = build(emit_p4, pk.items, irows)
    return run(_CACHE["p4"], maps)


def host_p5(mod, l, p3, p4):
    fps = [np.asarray(r["FP"]) for r in p4]
    maps = []
    for j in range(NCORE):
        fp = np.stack([np.concatenate([f[j * TL:(j + 1) * TL], f[S:S + CT]], axis=0) for f in fps])
        pk = Packer()
        pk.add("modv", mod[:, l, :].reshape(2, 6, 1024))
        arr, irows = pk.finish_single()
        maps.append({"inp": arr, "fp": fp, "x1": np.asarray(p3[j]["X1"])})
    if "p5" not in _CACHE: _CACHE["p5"] = build(emit_p5, pk.items, irows)
    return run(_CACHE["p5"], maps)


def kernel(**inputs):
    inp = {k: np.asarray(v) for k, v in inputs.items()}
    mod = host_p0(inp)
    x = np.asarray(inp["x"], np.float32).reshape(S, D)
    xc = np.asarray(inp["ctx"], np.float32).reshape(CT, D)
    for l in range(L):
        p1 = host_p1(inp, l, x, xc, mod)
        p2 = host_p2(inp, l, p1)
        p3 = host_p3(inp, l, x, xc, mod, p1, p2)
        del p2
        p4 = host_p4(inp, l, p3)
        p5 = host_p5(mod, l, p3, p4)
        del p1, p3, p4
        x = np.concatenate([np.asarray(r["X2"])[:TL] for r in p5], axis=0)
        xc = np.ascontiguousarray(np.asarray(p5[0]["X2"])[TL:])
    return np.ascontiguousarray(x, dtype=np.float32).reshape(1, S, D)
```
